# Optimizing a Trainium2 kernel written in Bass

```python
import jax, jax.numpy as jnp
from jax import lax
import numpy as np

D_MODEL = 2048
BATCH = 4
SEQ = 2048
DEPTH = 2

N_A_LAYERS = DEPTH // 2
N_B_LAYERS = DEPTH - N_A_LAYERS
CONV_WIDTH = 3
N_HEADS = 16
HEAD_DIM = D_MODEL // N_HEADS
ROT_DIM = HEAD_DIM // 4
ROPE_THETA = 500000.0
MOBA_BLOCK = 256
MOBA_TOPK = 3
Q_CHUNK = 16
N_EXPERTS = 32
TOP_K = 4
D_FF = D_MODEL
SWIGLU_ALPHA = 1.702
SWIGLU_LIMIT = 7.0
NORM_EPS = 1e-5
NEG_INF = -1e30
N_ADA = 6

kernel_name = "yoco_shortconv_moba_moe_adaln"


def rmsnorm(x, g):
    xf = x.astype(jnp.float32)
    xf = xf * lax.rsqrt(jnp.mean(xf * xf, axis=-1, keepdims=True) + NORM_EPS)
    return xf.astype(x.dtype) * g


def modulate(x, g, shift, scale):
    return rmsnorm(x, g) * (1 + scale) + shift


def rope_tables(positions):
    inv = jnp.float32(ROPE_THETA) ** (-jnp.arange(0, ROT_DIM, 2, dtype=jnp.float32) / ROT_DIM)
    ang = positions.astype(jnp.float32)[..., None] * inv
    return jnp.cos(ang)[:, :, None, :], jnp.sin(ang)[:, :, None, :]


def apply_rope(x, cos, sin):
    half = ROT_DIM // 2
    x1 = x[..., :half].astype(jnp.float32)
    x2 = x[..., half:ROT_DIM].astype(jnp.float32)
    rot = jnp.concatenate([x1 * cos - x2 * sin, x2 * cos + x1 * sin], axis=-1).astype(x.dtype)
    return jnp.concatenate([rot, x[..., ROT_DIM:]], axis=-1)


def short_conv_mixer(h, w_in, w_conv, w_out):
    seq = h.shape[1]
    b_gate, c_gate, u = jnp.split(h @ w_in, 3, axis=-1)
    v = c_gate * u
    vp = jnp.pad(v, ((0, 0), (CONV_WIDTH - 1, 0), (0, 0)))
    conv = sum(w_conv[j] * vp[:, j:j + seq] for j in range(CONV_WIDTH))
    return (b_gate * conv) @ w_out


def shared_kv(x, kv_norm_g, w_kv, cos, sin):
    bsz, seq, _ = x.shape
    kv = (rmsnorm(x, kv_norm_g) @ w_kv).reshape(bsz, seq, 2, N_HEADS, HEAD_DIM)
    k = apply_rope(kv[:, :, 0], cos, sin).transpose(0, 2, 1, 3)
    v = kv[:, :, 1].transpose(0, 2, 1, 3)
    n_blocks = -(-seq // MOBA_BLOCK)
    pad = n_blocks * MOBA_BLOCK - seq
    k_pad = jnp.pad(k, ((0, 0), (0, 0), (0, pad), (0, 0)))
    v_pad = jnp.pad(v, ((0, 0), (0, 0), (0, pad), (0, 0)))
    k_mean = k_pad.astype(jnp.float32).reshape(bsz, N_HEADS, n_blocks, MOBA_BLOCK, HEAD_DIM).mean(axis=3)
    return k_pad, v_pad, k_mean


def moba_attention(h, w_q, w_o, k_pad, v_pad, k_mean, cos, sin):
    bsz, seq, _ = h.shape
    n_blocks = k_mean.shape[2]
    topk = min(MOBA_TOPK, n_blocks)
    q = apply_rope((h @ w_q).reshape(bsz, seq, N_HEADS, HEAD_DIM), cos, sin).transpose(0, 2, 1, 3)
    k_blk = k_pad.reshape(bsz, N_HEADS, n_blocks, MOBA_BLOCK, HEAD_DIM)
    v_blk = v_pad.reshape(bsz, N_HEADS, n_blocks, MOBA_BLOCK, HEAD_DIM)
    b_ix = jnp.arange(bsz)[:, None, None, None]
    h_ix = jnp.arange(N_HEADS)[None, :, None, None]
    scale = HEAD_DIM ** -0.5

    def attend_chunk(ci):
        q0 = ci * Q_CHUNK
        blk = q0 // MOBA_BLOCK
        q_c = lax.dynamic_slice_in_dim(q, q0, Q_CHUNK, axis=2)
        gate = jnp.einsum('bhqd,bhnd->bhqn', q_c.astype(jnp.float32), k_mean)
        gate = jnp.where(jnp.arange(n_blocks) < blk, gate, NEG_INF)
        _, sel = lax.top_k(gate, topk)
        sel_valid = sel < blk
        k_sel = k_blk[b_ix, h_ix, sel]
        v_sel = v_blk[b_ix, h_ix, sel]
        k_own = lax.dynamic_slice_in_dim(k_pad, blk * MOBA_BLOCK, MOBA_BLOCK, axis=2)
        v_own = lax.dynamic_slice_in_dim(v_pad, blk * MOBA_BLOCK, MOBA_BLOCK, axis=2)
        s_sel = jnp.einsum('bhqd,bhqjkd->bhqjk', q_c, k_sel, preferred_element_type=jnp.float32) * scale
        s_sel = jnp.where(sel_valid[..., None], s_sel, NEG_INF).reshape(bsz, N_HEADS, Q_CHUNK, topk * MOBA_BLOCK)
        s_own = jnp.einsum('bhqd,bhkd->bhqk', q_c, k_own, preferred_element_type=jnp.float32) * scale
        q_pos = q0 + jnp.arange(Q_CHUNK)
        k_pos = blk * MOBA_BLOCK + jnp.arange(MOBA_BLOCK)
        s_own = jnp.where(k_pos[None, :] <= q_pos[:, None], s_own, NEG_INF)
        p = jax.nn.softmax(jnp.concatenate([s_sel, s_own], axis=-1), axis=-1).astype(v_pad.dtype)
        p_sel = p[..., :topk * MOBA_BLOCK].reshape(bsz, N_HEADS, Q_CHUNK, topk, MOBA_BLOCK)
        p_own = p[..., topk * MOBA_BLOCK:]
        return (jnp.einsum('bhqjk,bhqjkd->bhqd', p_sel, v_sel)
                + jnp.einsum('bhqk,bhkd->bhqd', p_own, v_own))

    out = lax.map(attend_chunk, jnp.arange(seq // Q_CHUNK))
    out = out.transpose(1, 0, 3, 2, 4).reshape(bsz, seq, N_HEADS * HEAD_DIM)
    return out @ w_o


def moe_ffn(h, w_router, b_router, w_gu, b_gu, w_down, b_down):
    bsz, seq, d = h.shape
    t = h.reshape(bsz * seq, d)
    logits = (t @ w_router + b_router).astype(jnp.float32)
    top_val, top_idx = lax.top_k(logits, TOP_K)
    top_w = jax.nn.softmax(top_val, axis=-1)
    comb = jnp.sum(jax.nn.one_hot(top_idx, N_EXPERTS, dtype=jnp.float32) * top_w[..., None], axis=-2)
    comb = comb.astype(h.dtype)

    def expert_step(acc, params):
        wgu, bgu, wd, bd, g = params
        gu = t @ wgu + bgu
        gate = jnp.minimum(gu[:, :D_FF], SWIGLU_LIMIT)
        up = jnp.clip(gu[:, D_FF:], -SWIGLU_LIMIT, SWIGLU_LIMIT)
        glu = gate * jax.nn.sigmoid(gate * SWIGLU_ALPHA)
        out = ((up + 1) * glu) @ wd + bd
        return acc + g[:, None] * out, None

    y, _ = lax.scan(expert_step, jnp.zeros_like(t), (w_gu, b_gu, w_down, b_down, comb.T))
    return y.reshape(bsz, seq, d)


def setup_inputs(seed: int = 0) -> dict:
    key = jax.random.key(seed)
    ks = jax.random.split(key, 20)
    d, hd = D_MODEL, N_HEADS * HEAD_DIM

    def nrm(k, shape, s):
        return jax.random.normal(k, shape, jnp.float32) * s

    return {
        "x": nrm(ks[0], (BATCH, SEQ, d), 1.0),
        "c": nrm(ks[1], (BATCH, d), 1.0),
        "positions": jnp.broadcast_to(jnp.arange(SEQ, dtype=jnp.int32), (BATCH, SEQ)),
        "norm_g": 1.0 + nrm(ks[2], (DEPTH, 2, d), 0.1),
        "ada_w": nrm(ks[3], (DEPTH, d, N_ADA * d), 0.5 * d ** -0.5),
        "ada_b": nrm(ks[4], (DEPTH, N_ADA * d), 0.02),
        "conv_w_in": nrm(ks[5], (N_A_LAYERS, d, 3 * d), d ** -0.5),
        "conv_w": nrm(ks[6], (N_A_LAYERS, CONV_WIDTH, d), CONV_WIDTH ** -0.5),
        "conv_w_out": nrm(ks[7], (N_A_LAYERS, d, d), d ** -0.5),
        "kv_norm_g": 1.0 + nrm(ks[8], (d,), 0.1),
        "w_kv": nrm(ks[9], (d, 2 * hd), d ** -0.5),
        "attn_w_q": nrm(ks[10], (N_B_LAYERS, d, hd), d ** -0.5),
        "attn_w_o": nrm(ks[11], (N_B_LAYERS, hd, d), hd ** -0.5),
        "router_w": nrm(ks[12], (DEPTH, d, N_EXPERTS), d ** -0.5),
        "router_b": nrm(ks[13], (DEPTH, N_EXPERTS), 0.01),
        "moe_w_gu": nrm(ks[14], (DEPTH, N_EXPERTS, d, 2 * D_FF), d ** -0.5),
        "moe_b_gu": nrm(ks[15], (DEPTH, N_EXPERTS, 2 * D_FF), 0.01),
        "moe_w_down": nrm(ks[16], (DEPTH, N_EXPERTS, D_FF, d), D_FF ** -0.5),
        "moe_b_down": nrm(ks[17], (DEPTH, N_EXPERTS, d), 0.01),
        "final_g": 1.0 + nrm(ks[18], (d,), 0.1),
    }


def reference(x, c, positions, norm_g, ada_w, ada_b, conv_w_in, conv_w, conv_w_out,
              kv_norm_g, w_kv, attn_w_q, attn_w_o, router_w, router_b,
              moe_w_gu, moe_b_gu, moe_w_down, moe_b_down, final_g):
    cos, sin = rope_tables(positions)
    c_act = jax.nn.silu(c)
    k_pad = v_pad = k_mean = None
    for layer in range(DEPTH):
        ada = (c_act @ ada_w[layer] + ada_b[layer])[:, None, :]
        sh1, sc1, gt1, sh2, sc2, gt2 = jnp.split(ada, N_ADA, axis=-1)
        h = modulate(x, norm_g[layer, 0], sh1, sc1)
        if layer < N_A_LAYERS:
            mix = short_conv_mixer(h, conv_w_in[layer], conv_w[layer], conv_w_out[layer])
        else:
            if layer == N_A_LAYERS:
                k_pad, v_pad, k_mean = shared_kv(x, kv_norm_g, w_kv, cos, sin)
            j = layer - N_A_LAYERS
            mix = moba_attention(h, attn_w_q[j], attn_w_o[j], k_pad, v_pad, k_mean, cos, sin)
        x = x + gt1 * mix
        h = modulate(x, norm_g[layer, 1], sh2, sc2)
        x = x + gt2 * moe_ffn(h, router_w[layer], router_b[layer], moe_w_gu[layer], moe_b_gu[layer],
                              moe_w_down[layer], moe_b_down[layer])
    return rmsnorm(x, final_g)
```

```python
import numpy as np
import ml_dtypes
from contextlib import ExitStack
import concourse.bass as bass
import concourse.mybir as mybir
from concourse.bass_utils import run_bass_kernel_spmd

F32 = mybir.dt.float32
BF16 = mybir.dt.bfloat16
I32 = mybir.dt.int32
ALU = mybir.AluOpType
AF = mybir.ActivationFunctionType
AX = mybir.AxisListType

NCORES = 8
NT = 1024
D = 2048
KC = 16
E = 32
EL = 4
CAP = 2048
NH = 16
DH = 128
_uid = [0]


def uid():
    _uid[0] += 1
    return _uid[0]


class Chan:
    def __init__(self, sem, name):
        self.sem = sem
        self.name = name
        self.count = 0

    def tok(self):
        return (self.sem, self.count, self.name)


class Ctx:
    ENG = ('pe', 'act', 'dve', 'pool', 'sp')

    def __init__(self, nc, es):
        self.nc = nc
        self.es = es
        u = uid()
        self.sem = {k: es.enter_context(nc.semaphore(f"s{u}_{k}")) for k in self.ENG}
        self.cnt = {k: 0 for k in self.ENG}
        self.seen = {k: {} for k in self.ENG}
        self.ops = {k: [] for k in self.ENG}
        self.u = u
        self.nchan = 0
        self.regcache = {}

    def sb(self, name, shape, dtype):
        return self.es.enter_context(self.nc.sbuf_tensor(f"{name}_{self.u}", list(shape), dtype))

    def ps(self, name, shape, dtype=F32):
        return self.es.enter_context(self.nc.psum_tensor(f"{name}_{self.u}", list(shape), dtype))

    def chan(self, name="c"):
        self.nchan += 1
        nm = f"d{self.u}_{name}{self.nchan}"
        return Chan(self.es.enter_context(self.nc.semaphore(nm)), nm)

    def _waits(self, e, deps):
        waits = []
        flat = []

        def _fl(d):
            for x in d:
                if x is None:
                    continue
                if isinstance(x, list):
                    _fl(x)
                else:
                    flat.append(x)
        _fl(list(deps))
        for tok in flat:
            sem, val, name = tok
            if val <= 0:
                continue
            if self.seen[e].get(name, 0) >= val:
                continue
            self.seen[e][name] = val
            waits.append((sem, val))
        return waits

    def op(self, e, fn, deps=(), mark=True):
        waits = self._waits(e, deps)
        tok = None
        if mark:
            self.cnt[e] += 1
            tok = (self.sem[e], self.cnt[e], e)
        sem = self.sem[e]

        def emit(engine):
            for s, v in waits:
                engine.wait_ge(s, v)
            ins = fn(engine)
            if mark:
                ins.then_inc(sem, 1)
        self.ops[e].append(emit)
        return tok

    def dma(self, q, ch, out, in_, deps=(), indirect=None, **kw):
        deps = list(deps) + [ch.tok()]
        waits = self._waits(q, deps)
        ch.count += 16
        tok = ch.tok()
        sem = ch.sem

        def emit(engine):
            for s, v in waits:
                engine.wait_ge(s, v)
            if indirect is None:
                ins = engine.dma_start(out=out, in_=in_, **kw)
            else:
                ind = dict(indirect)
                if ind.get('bounds_check') is not None:
                    key = ('bc', ind['bounds_check'])
                    if key not in self.regcache:
                        self.regcache[key] = engine.to_reg(ind['bounds_check'])
                    ind['bounds_check'] = self.regcache[key]
                ins = engine.indirect_dma_start(out=out, in_=in_, **ind)
            ins.then_inc(sem, 16)
        self.ops[q].append(emit)
        return tok

    def wait(self, e, deps):
        waits = self._waits(e, deps)

        def emit(engine):
            for s, v in waits:
                engine.wait_ge(s, v)
        if waits:
            self.ops[e].append(emit)

    def flush(self, final_deps=()):
        self.wait('sp', final_deps)
        nc = self.nc
        ops = self.ops
        with nc.Block() as block:
            @block.tensor
            def _(eng):
                for f in ops['pe']:
                    f(eng)

            @block.scalar
            def _(eng):
                for f in ops['act']:
                    f(eng)

            @block.vector
            def _(eng):
                for f in ops['dve']:
                    f(eng)

            @block.gpsimd
            def _(eng):
                for f in ops['pool']:
                    f(eng)

            @block.sync
            def _(eng):
                for f in ops['sp']:
                    f(eng)


class Banks:
    def __init__(self, K, n=8):
        self.K = K
        self.t = [K.ps(f"bk{i}", [128, 512], F32) for i in range(n)]
        self.free = [None] * n

    def f32(self, i):
        return self.t[i][:]

    def bf16(self, i):
        return self.t[i][:].bitcast(BF16)


class WRing:
    def __init__(self, K, name, nslots, shape, dtype=BF16):
        self.K = K
        self.buf = K.sb(name, [128, nslots] + list(shape), dtype)
        self.n = nslots
        self.ch = [K.chan(name) for _ in range(nslots)]
        self.free = [None] * nslots
        self.i = 0

    def load(self, src_ap, q='pool', sub=None):
        s = self.i % self.n
        self.i += 1
        dst = self.buf[:, s] if sub is None else sub(self.buf[:, s])
        tok = self.K.dma(q, self.ch[s], dst, src_ap, deps=[self.free[s]])
        return s, self.buf[:, s], tok

    def release(self, s, tok):
        self.free[s] = tok


def wpiece(W, c0, w):
    return W[:, c0:c0 + w].rearrange("(kc p) f -> p kc f", p=128)


def load_consts(K, T):
    C = {}
    ch = K.chan("cst")
    C['ident32'] = K.sb("ident32", [128, 128], F32)
    C['identb'] = K.sb("identb", [128, 128], BF16)
    C['ones32'] = K.sb("ones32", [128, 128], F32)
    t1 = K.dma('sp', ch, C['ident32'][:], T['ident'])
    C['t_ident32'] = t1
    C['t_identb'] = K.op('dve', lambda e: e.tensor_copy(out=C['identb'][:], in_=C['ident32'][:]), deps=[t1])
    C['t_ones32'] = K.op('dve', lambda e: e.memset(C['ones32'][:], 1.0))
    return C


def featmajor_rows(K, C, BK, bank, rows_ap, R, nch, out_sb, pre=None, rw=None, deps=()):
    if rw is None:
        rw = K.sb(f"rw{uid()}", [R, nch * 128], F32)[:]
    ch = K.chan("rw")
    t = K.dma('sp', ch, rw[0:R, :], rows_ap, deps=list(deps))
    if pre is not None:
        t = pre(rw, t)
    ps = BK.f32(bank)[:, 0:nch * R].rearrange("p (a b) -> p a b", a=nch)
    tp = None
    for kc in range(nch):
        tp = K.op('pe', lambda e, kc=kc: e.transpose(out=ps[:, kc, :], in_=rw[0:R, kc * 128:(kc + 1) * 128],
                                                     identity=C['ident32'][0:R, 0:R]),
                  deps=[t, C['t_ident32'], BK.free[bank]], mark=(kc == nch - 1))
    tc_ = K.op('dve', lambda e: e.tensor_copy(out=out_sb, in_=ps), deps=[tp])
    BK.free[bank] = tc_
    return tc_


def load_ada(K, C, BK, bank, T, adaT):
    bsel = K.sb("bselc", [4, 1], F32)
    chb = K.chan("bsel")
    tb = K.dma('sp', chb, bsel[:], T['bselc'])
    gsb = K.sb("gsb", [4, 2, 512], F32)
    chg = [K.chan("g"), K.chan("g")]
    gfree = [None, None]
    ps = BK.f32(bank)[:, 0:192].rearrange("p (l c) -> p l c", l=2)
    tp = None
    i = 0
    for r in range(8):
        for l in range(2):
            row0 = r * 8 + l * 4
            for n in range(3):
                s = i % 2
                i += 1
                tg = K.dma('sp', chg[s], gsb[:, s, :], T['G'][row0:row0 + 4, n * 512:(n + 1) * 512], deps=[gfree[s]])
                for c4 in range(4):
                    cc = 4 * n + c4
                    tp = K.op('pe', lambda e, s=s, cc=cc, c4=c4, l=l, r=r: e.matmul(
                        ps[:, l, 12 * r + cc:12 * r + cc + 1], lhsT=gsb[0:4, s, c4 * 128:(c4 + 1) * 128], rhs=bsel[0:4, 0:1],
                        start=True, stop=True), deps=[tg, tb, BK.free[bank]], mark=(c4 == 3))
                gfree[s] = tp
    t = K.op('dve', lambda e: e.tensor_copy(out=adaT[:], in_=ps), deps=[tp])
    BK.free[bank] = t
    return t


def rms_rstd(K, C, BK, banks2, xT, ncol, rstd, sqbuf, deps):
    nh = (ncol + 511) // 512
    tsq = [None, None]
    sqfree = [None, None]
    tm = None
    for kc in range(KC):
        s = kc % 2
        tsq[s] = K.op('act', lambda e, kc=kc, s=s: e.activation(out=sqbuf[:, s, 0:ncol], in_=xT[:, kc, 0:ncol], func=AF.Square),
                      deps=list(deps) + [sqfree[s]])
        for h in range(nh):
            w = min(512, ncol - h * 512)
            tm = K.op('pe', lambda e, kc=kc, s=s, h=h, w=w: e.matmul(
                BK.f32(banks2[h])[:, 0:w], lhsT=C['ones32'][:], rhs=sqbuf[:, s, h * 512:h * 512 + w],
                start=(kc == 0), stop=(kc == KC - 1)),
                deps=[tsq[s], C['t_ones32'], BK.free[banks2[h]]], mark=(h == nh - 1))
        sqfree[s] = tm
    t = None
    for h in range(nh):
        w = min(512, ncol - h * 512)
        t1 = K.op('dve', lambda e, h=h, w=w: e.tensor_scalar(out=rstd[:, h * 512:h * 512 + w], in0=BK.f32(banks2[h])[:, 0:w],
                                                             scalar1=1.0 / D, scalar2=1e-5, op0=ALU.mult, op1=ALU.add), deps=[tm])
        BK.free[banks2[h]] = t1
        t2 = K.op('act', lambda e, h=h, w=w: e.activation(out=rstd[:, h * 512:h * 512 + w], in_=rstd[:, h * 512:h * 512 + w], func=AF.Sqrt), deps=[t1])
        t = K.op('dve', lambda e, h=h, w=w: e.reciprocal(out=rstd[:, h * 512:h * 512 + w], in_=rstd[:, h * 512:h * 512 + w]), deps=[t2, t])
    return t


def phase0(nc, T):
    with ExitStack() as es:
        K = Ctx(nc, es)
        C = load_consts(K, T)
        BK = Banks(K, 4)
        cT = K.sb("cT", [128, KC, 4], F32)

        def pre(rw, t):
            return K.op('act', lambda e: e.activation(out=rw[0:4, :], in_=rw[0:4, :], func=AF.Silu), deps=[t])
        t_c = featmajor_rows(K, C, BK, 3, T['c_all'], 4, KC, cT[:], pre=pre)
        wsb = K.sb("wsb", [128, 3, 1536], F32)
        chw = [K.chan("w") for _ in range(3)]
        wfree = [None] * 3
        osb = K.sb("osb", [4, 2, 1536], F32)
        bsb = K.sb("bsb", [4, 2, 1536], F32)
        chb = K.chan("b")
        cho = K.chan("o")
        for l in range(2):
            tb = K.dma('sp', chb, bsb[:, l, :], T['ada_b_sh'][l:l + 1, :].to_broadcast([4, 1536]))
            tm = None
            for kc in range(KC):
                i = l * KC + kc
                s = i % 3
                tw = K.dma('sp', chw[s], wsb[:, s, :], T['ada_w_sh'][l, kc * 128:(kc + 1) * 128, :], deps=[wfree[s]])
                for n in range(3):
                    tm = K.op('pe', lambda e, kc=kc, s=s, n=n: e.matmul(
                        BK.f32(n)[0:4, :], lhsT=cT[:, kc, :], rhs=wsb[:, s, n * 512:(n + 1) * 512],
                        start=(kc == 0), stop=(kc == KC - 1)), deps=[tw, t_c, BK.free[n]], mark=(n == 2))
                wfree[s] = tm
            to = None
            for n in range(3):
                to = K.op('dve', lambda e, n=n, l=l: e.tensor_tensor(out=osb[:, l, n * 512:(n + 1) * 512], in0=BK.f32(n)[0:4, :],
                                                                      in1=bsb[:, l, n * 512:(n + 1) * 512], op=ALU.add), deps=[tm, tb])
                BK.free[n] = to
            K.dma('sp', cho, T['ada_part'][l * 4:(l + 1) * 4, :], osb[:, l, :], deps=[to])
        K.flush(final_deps=[cho.tok()])


def moe_home_pre(K, C, BK, T, xT, adaT, featT, layer, hbuf, scratch, t_x):
    gcol = 1 + 2 * layer
    gs2 = K.sb("gs2", [128, KC], F32)
    t_g = K.op('dve', lambda e: e.scalar_tensor_tensor(out=gs2[:], in0=adaT[:, layer, 64:80], scalar=1.0, in1=featT[:, :, gcol],
                                                       op0=ALU.add, op1=ALU.mult), deps=[t_x])
    rstd = scratch['rstd']
    t_r = rms_rstd(K, C, BK, [0, 1], xT, NT, rstd, scratch['sq'], [t_x])
    wr = K.sb("wr", [128, KC, E], F32)
    chr_ = K.chan("wr")
    t_wr = K.dma('sp', chr_, wr[:], T['router_w'][layer].rearrange("(kc p) e -> p kc e", p=128))
    lg = K.sb("lg", [128, 8, E], F32)
    t_rb = K.dma('sp', chr_, lg[:], T['router_b'][layer:layer + 1, :].unsqueeze(1).to_broadcast([128, 8, E]))
    h32 = scratch['h32']
    tmp = scratch['sq']
    lb = [2, 7]
    h32free = [None, None]
    tmpfree = [None, None]
    t_hb = None
    t_lg = t_rb
    for kc in range(KC):
        s = kc % 2
        t1 = K.op('dve', lambda e, kc=kc, s=s: e.tensor_tensor(out=tmp[:, s, :], in0=xT[:, kc, :], in1=rstd[:, 0:NT], op=ALU.mult),
                  deps=[t_r, tmpfree[s]])
        t2 = K.op('act', lambda e, kc=kc, s=s: e.activation(out=h32[:, s, :], in_=tmp[:, s, :], func=AF.Identity,
                                                            scale=gs2[:, kc:kc + 1], bias=adaT[:, layer, 48 + kc:49 + kc]),
                  deps=[t1, t_g, h32free[s]])
        tmpfree[s] = t2
        t_hb = K.op('dve', lambda e, kc=kc, s=s: e.tensor_copy(out=hbuf[:, kc, 0:NT], in_=h32[:, s, :]), deps=[t2])
        bk = lb[s]
        ps_log = BK.f32(bk)[:, 0:256].rearrange("p (t e) -> p t e", t=8)
        tl = None
        for tt in range(8):
            tl = K.op('pe', lambda e, kc=kc, s=s, tt=tt, ps_log=ps_log: e.matmul(ps_log[:, tt, :], lhsT=h32[:, s, tt * 128:(tt + 1) * 128], rhs=wr[:, kc, :],
                                                                             start=True, stop=True),
                      deps=[t2, t_wr, BK.free[bk]], mark=(tt == 7))
        t_lg = K.op('dve', lambda e, ps_log=ps_log: e.tensor_tensor(out=lg[:], in0=lg[:], in1=ps_log, op=ALU.add), deps=[tl, t_lg])
        BK.free[bk] = t_lg
        h32free[s] = [tl, t_hb]
        scratch.setdefault('_hb', []).append(t_hb)
    top8 = K.sb("top8", [128, 8, 8], F32)
    t8 = None
    for tt in range(8):
        t8 = K.op('dve', lambda e, tt=tt: e.max(out=top8[:, tt, :], in_=lg[:, tt, :]), deps=[t_lg])
    mask = K.sb("rmask", [128, 8, E], F32)
    t_m = K.op('dve', lambda e: e.tensor_tensor(out=mask[:], in0=lg[:], in1=top8[:, :, 3:4].to_broadcast([128, 8, E]), op=ALU.is_ge), deps=[t8])
    ex = K.sb("rex", [128, 8, E], F32)
    t_e = K.op('dve', lambda e: e.tensor_tensor(out=ex[:], in0=lg[:], in1=top8[:, :, 0:1].to_broadcast([128, 8, E]), op=ALU.subtract), deps=[t8])
    t_e = K.op('act', lambda e: e.activation(out=ex[:], in_=ex[:], func=AF.Exp), deps=[t_e])
    t_e = K.op('dve', lambda e: e.tensor_tensor(out=ex[:], in0=ex[:], in1=mask[:], op=ALU.mult), deps=[t_e, t_m])
    rs = K.sb("rrs", [128, 8], F32)
    t_s = K.op('dve', lambda e: e.tensor_reduce(out=rs[:], in_=ex[:], axis=AX.X, op=ALU.add), deps=[t_e])
    t_s = K.op('dve', lambda e: e.reciprocal(out=rs[:], in_=rs[:]), deps=[t_s])
    t_c = K.op('dve', lambda e: e.tensor_tensor(out=ex[:], in0=ex[:], in1=rs[:].unsqueeze(2).to_broadcast([128, 8, E]), op=ALU.mult), deps=[t_s])
    cho = K.chan("co")
    K.dma('sp', cho, T['comb_loc'].rearrange("(t p) e -> p t e", p=128), ex[:], deps=[t_c])
    hrow = scratch['hrow'] if 'hrow' in scratch else K.sb("hrow", [128, 2, D], BF16)
    chh = [K.chan("hr"), K.chan("hr")]
    hrfree = [None, None]
    t_hb_all = scratch['_hb'][-1]
    pb = [3, 4, 5, 6]
    for tt in range(8):
        s = tt % 2
        for half in range(2):
            b = pb[(tt * 2 + half) % 4]
            psb = BK.bf16(b).rearrange("p (a c) -> p a c", a=8)
            tp = None
            for j in range(8):
                kc = half * 8 + j
                tp = K.op('pe', lambda e, kc=kc, j=j, tt=tt, psb=psb: e.transpose(out=psb[:, j, :], in_=hbuf[:, kc, tt * 128:(tt + 1) * 128],
                                                                                identity=C['identb'][:]),
                          deps=[t_hb_all, C['t_identb'], BK.free[b]], mark=(j == 7))
            eng = 'act' if half == 0 else 'dve'
            if eng == 'act':
                tcp = K.op('act', lambda e, s=s, half=half, b=b: e.activation(out=hrow[:, s, half * 1024:(half + 1) * 1024], in_=BK.bf16(b), func=AF.Copy),
                           deps=[tp, hrfree[s]])
            else:
                tcp = K.op('dve', lambda e, s=s, half=half, b=b: e.tensor_copy(out=hrow[:, s, half * 1024:(half + 1) * 1024], in_=BK.bf16(b)),
                           deps=[tp, hrfree[s]])
            BK.free[b] = tcp
            if half == 0:
                tcp0 = tcp
        hrfree[s] = K.dma('sp', chh[s], T['h2_loc'][tt * 128:(tt + 1) * 128, :], hrow[:, s, :], deps=[tcp0, tcp])
    chs = K.chan("sp")
    K.dma('sp', chs, T['xT_spill'].rearrange("p (k t) -> p k t", k=KC), xT[:], deps=[t_x])
    return [cho.tok(), chh[0].tok(), chh[1].tok(), chs.tok()]


def phase1(nc, T):
    with ExitStack() as es:
        K = Ctx(nc, es)
        C = load_consts(K, T)
        BK = Banks(K, 8)
        xT = K.sb("xT", [128, KC, NT], F32)
        hT = K.sb("hT", [128, KC, NT + 2], BF16)
        zT = K.sb("zT", [128, KC, NT], BF16)
        featT = K.sb("featT", [128, KC, 15], F32)
        adaT = K.sb("adaT", [128, 2, 96], F32)
        scratch = {'rstd': K.sb("rstd", [128, NT + 2], F32), 'sq': K.sb("sq", [128, 2, NT], F32),
                   'h32': K.sb("h32", [128, 2, NT], F32)}
        t_feat = featmajor_rows(K, C, BK, 7, T['smallpack'], 15, KC, featT[:], rw=scratch['sq'][:].rearrange("p a b -> p (a b)"))
        t_ada = load_ada(K, C, BK, 6, T, adaT)
        scratch['hrow'] = zT[:].rearrange("p k t -> p (k t)")[:, 0:2 * D].rearrange("p (s d) -> p s d", s=2)
        xin = scratch['h32']
        chx = K.chan("x")
        xfree = None
        t_x = None
        for tt in range(8):
            tl = K.dma('sp', chx, xin[:].rearrange("p a b -> p (a b)"), T['x_loc'][tt * 128:(tt + 1) * 128, :], deps=[xfree])
            xflat = xin[:].rearrange("p a b -> p (a b)")
            for g in range(4):
                b = g % 2
                ps = BK.f32(b).rearrange("p (a c) -> p a c", a=4)
                tp = None
                for j in range(4):
                    kc = 4 * g + j
                    tp = K.op('pe', lambda e, kc=kc, j=j, ps=ps: e.transpose(out=ps[:, j, :], in_=xflat[:, kc * 128:(kc + 1) * 128], identity=C['ident32'][:]),
                              deps=[tl, C['t_ident32'], BK.free[b]], mark=(j == 3))
                if g % 2 == 0:
                    t_x = K.op('act', lambda e, g=g, tt=tt, ps=ps: e.activation(out=xT[:, 4 * g:4 * g + 4, tt * 128:(tt + 1) * 128], in_=ps, func=AF.Copy), deps=[tp])
                else:
                    t_x = K.op('dve', lambda e, g=g, tt=tt, ps=ps: e.tensor_copy(out=xT[:, 4 * g:4 * g + 4, tt * 128:(tt + 1) * 128], in_=ps), deps=[tp])
                BK.free[b] = t_x
            xfree = tp
        t_xall = [t_x, BK.free[0], BK.free[1], t_feat]
        gs1 = K.sb("gs1", [128, KC], F32)
        t_g = K.op('dve', lambda e: e.scalar_tensor_tensor(out=gs1[:], in0=adaT[:, 0, 16:32], scalar=1.0, in1=featT[:, :, 0],
                                                           op0=ALU.add, op1=ALU.mult), deps=[t_ada, t_feat])
        rstd = scratch['rstd']
        t_r = rms_rstd(K, C, BK, [0, 1], xT, NT, rstd, scratch['sq'], t_xall)
        sqh = K.sb("sqh", [128, KC, 2], F32)
        t_sh = K.op('dve', lambda e: e.tensor_tensor(out=sqh[:], in0=featT[:, :, 9:11], in1=featT[:, :, 9:11], op=ALU.mult), deps=[t_feat])
        tm = None
        for kc in range(KC):
            tm = K.op('pe', lambda e, kc=kc: e.matmul(BK.f32(2)[:, 0:2], lhsT=C['ones32'][:], rhs=sqh[:, kc, :], start=(kc == 0), stop=(kc == KC - 1)),
                      deps=[t_sh, C['t_ones32']], mark=(kc == KC - 1))
        rh = K.sb("rh", [128, 2], F32)
        t1 = K.op('dve', lambda e: e.tensor_scalar(out=rh[:], in0=BK.f32(2)[:, 0:2], scalar1=1.0 / D, scalar2=1e-5, op0=ALU.mult, op1=ALU.add), deps=[tm])
        BK.free[2] = t1
        t1 = K.op('act', lambda e: e.activation(out=rh[:], in_=rh[:], func=AF.Sqrt), deps=[t1])
        t_rh = K.op('dve', lambda e: e.reciprocal(out=rh[:], in_=rh[:]), deps=[t1])
        hh = K.sb("hh", [128, KC, 2], F32)
        t2 = K.op('dve', lambda e: e.tensor_tensor(out=hh[:], in0=featT[:, :, 9:11], in1=rh[:].unsqueeze(1).to_broadcast([128, KC, 2]), op=ALU.mult), deps=[t_rh])
        t2 = K.op('dve', lambda e: e.tensor_tensor(out=hh[:], in0=hh[:], in1=gs1[:].unsqueeze(2).to_broadcast([128, KC, 2]), op=ALU.mult), deps=[t2, t_g])
        t_hh = K.op('dve', lambda e: e.tensor_tensor(out=hT[:, :, 0:2], in0=hh[:], in1=adaT[:, 0, 0:16].unsqueeze(2).to_broadcast([128, KC, 2]), op=ALU.add), deps=[t2])
        tmp = scratch['sq']
        tmpfree = [None, None]
        t_h = None
        for kc in range(KC):
            s = kc % 2
            t1 = K.op('dve', lambda e, kc=kc, s=s: e.tensor_tensor(out=tmp[:, s, :], in0=xT[:, kc, :], in1=rstd[:, 0:NT], op=ALU.mult), deps=[t_r, tmpfree[s]])
            t_h = K.op('act', lambda e, kc=kc, s=s: e.activation(out=hT[:, kc, 2:NT + 2], in_=tmp[:, s, :], func=AF.Identity,
                                                                 scale=gs1[:, kc:kc + 1], bias=adaT[:, 0, kc:kc + 1]), deps=[t1, t_g])
            tmpfree[s] = t_h
        t_hT = [t_h, t_hh]
        ring = WRing(K, "wring", 3, [KC, 256])
        hflag = K.sb("hflag", [128, 1], F32)
        chf = K.chan("hf")
        t_hf = K.dma('sp', chf, hflag[:], T['haloflag'])
        cS = K.sb("cS", [128, 2, 512], F32)
        bS = K.sb("bS", [128, NT], F32)
        vT = K.sb("vT", [128, NT + 2], F32)
        acc = K.sb("acc", [128, NT], F32)
        cSfree = [None, None]
        vfree = None
        bSfree = None
        accfree = None
        w_in = T['conv_w_in']
        t_z = None
        for i in range(8):
            sc_, Wc, tWc = ring.load(wpiece(w_in, 2048 + 256 * i, 256))
            su_, Wu, tWu = ring.load(wpiece(w_in, 4096 + 256 * i, 256))
            sb_, Wb, tWb = ring.load(wpiece(w_in, 256 * i, 256))
            lastpe = None
            for j in range(2):
                fc = 2 * i + j
                for kc in range(KC):
                    K.op('pe', lambda e, kc=kc, j=j, Wc=Wc: e.matmul(BK.f32(6)[:, 0:2], lhsT=Wc[:, kc, j * 128:(j + 1) * 128], rhs=hT[:, kc, 0:2],
                                                                     start=(kc == 0), stop=(kc == KC - 1)), deps=[tWc, BK.free[6]] + t_hT, mark=False)
                th_ = None
                for kc in range(KC):
                    th_ = K.op('pe', lambda e, kc=kc, j=j, Wu=Wu: e.matmul(BK.f32(6)[:, 2:4], lhsT=Wu[:, kc, j * 128:(j + 1) * 128], rhs=hT[:, kc, 0:2],
                                                                           start=(kc == 0), stop=(kc == KC - 1)), deps=[tWu], mark=(kc == KC - 1))
                tv0 = K.op('act', lambda e: e.activation(out=cS[:, 0, 0:2], in_=BK.f32(6)[:, 0:2], func=AF.Copy), deps=[th_, cSfree[0]])
                tvh = K.op('dve', lambda e: e.tensor_tensor(out=vT[:, 0:2], in0=BK.f32(6)[:, 2:4], in1=cS[:, 0, 0:2], op=ALU.mult), deps=[tv0, vfree])
                tvh = K.op('dve', lambda e: e.tensor_scalar(out=vT[:, 0:2], in0=vT[:, 0:2], scalar1=hflag[:, 0:1], scalar2=None, op0=ALU.mult), deps=[tvh, t_hf])
                BK.free[6] = tvh
                cSfree[0] = tvh
                tvs = [tvh]
                tbs = []
                for half in range(2):
                    bC, bU, bB = half * 3, half * 3 + 1, half * 3 + 2
                    cols = slice(2 + half * 512, 2 + (half + 1) * 512)
                    for (bk, W, tW) in ((bC, Wc, tWc), (bU, Wu, tWu), (bB, Wb, tWb)):
                        for kc in range(KC):
                            lastpe = K.op('pe', lambda e, kc=kc, j=j, W=W, bk=bk, cols=cols: e.matmul(
                                BK.f32(bk), lhsT=W[:, kc, j * 128:(j + 1) * 128], rhs=hT[:, kc, cols], start=(kc == 0), stop=(kc == KC - 1)),
                                deps=[tW, BK.free[bk]] + t_hT, mark=(kc == KC - 1))
                        if bk == bC:
                            tC = lastpe
                        elif bk == bU:
                            tU = lastpe
                        else:
                            tB = lastpe
                    t_c = K.op('act', lambda e, half=half, bC=bC: e.activation(out=cS[:, half, :], in_=BK.f32(bC), func=AF.Copy), deps=[tC, cSfree[half]])
                    BK.free[bC] = t_c
                    t_v = K.op('dve', lambda e, half=half, bU=bU, cols=cols: e.tensor_tensor(out=vT[:, cols], in0=BK.f32(bU), in1=cS[:, half, :], op=ALU.mult),
                               deps=[tU, t_c, vfree])
                    BK.free[bU] = t_v
                    cSfree[half] = t_v
                    t_b = K.op('act', lambda e, half=half, bB=bB: e.activation(out=bS[:, half * 512:(half + 1) * 512], in_=BK.f32(bB), func=AF.Copy), deps=[tB, bSfree])
                    BK.free[bB] = t_b
                    tvs.append(t_v)
                    tbs.append(t_b)
                ta = K.op('dve', lambda e, fc=fc: e.tensor_scalar(out=acc[:], in0=vT[:, 2:NT + 2], scalar1=featT[:, fc, 6:7], scalar2=None, op0=ALU.mult),
                          deps=tvs + [accfree, t_feat])
                ta = K.op('dve', lambda e, fc=fc: e.scalar_tensor_tensor(out=acc[:], in0=vT[:, 1:NT + 1], scalar=featT[:, fc, 5:6], in1=acc[:], op0=ALU.mult, op1=ALU.add), deps=[ta])
                ta = K.op('dve', lambda e, fc=fc: e.scalar_tensor_tensor(out=acc[:], in0=vT[:, 0:NT], scalar=featT[:, fc, 4:5], in1=acc[:], op0=ALU.mult, op1=ALU.add), deps=[ta])
                vfree = ta
                t_z = K.op('dve', lambda e, fc=fc: e.tensor_tensor(out=zT[:, fc, :], in0=acc[:], in1=bS[:], op=ALU.mult), deps=[ta] + tbs)
                accfree = t_z
                bSfree = t_z
            ring.release(sc_, lastpe)
            ring.release(su_, lastpe)
            ring.release(sb_, lastpe)
        w_out = T['conv_w_out']
        t_xn = None
        xtoks = []
        for i in range(8):
            so_, Wo, tWo = ring.load(wpiece(w_out, 256 * i, 256))
            lastpe = None
            for j in range(2):
                dc = 2 * i + j
                for half in range(2):
                    bk = (dc * 2 + half) % 6
                    for kc in range(KC):
                        lastpe = K.op('pe', lambda e, kc=kc, j=j, Wo=Wo, bk=bk, half=half: e.matmul(
                            BK.f32(bk), lhsT=Wo[:, kc, j * 128:(j + 1) * 128], rhs=zT[:, kc, half * 512:(half + 1) * 512],
                            start=(kc == 0), stop=(kc == KC - 1)), deps=[tWo, t_z, BK.free[bk]], mark=(kc == KC - 1))
                    t_xn = K.op('dve', lambda e, dc=dc, half=half, bk=bk: e.scalar_tensor_tensor(
                        out=xT[:, dc, half * 512:(half + 1) * 512], in0=BK.f32(bk), scalar=adaT[:, 0, 32 + dc:33 + dc],
                        in1=xT[:, dc, half * 512:(half + 1) * 512], op0=ALU.mult, op1=ALU.add), deps=[lastpe, t_h])
                    BK.free[bk] = t_xn
            ring.release(so_, lastpe)
        toks = moe_home_pre(K, C, BK, T, xT, adaT, featT, 0, hT, scratch, t_xn)
        K.flush(final_deps=toks)


def phase_expert(nc, T, layer):
    with ExitStack() as es:
        K = Ctx(nc, es)
        C = load_consts(K, T)
        BK = Banks(K, 8)
        NTI = 64
        XT = K.sb("XT", [128, KC, 1024], BF16)
        actT = K.sb("actT", [128, KC, 1024], BF16)
        tmpm = XT[:].rearrange("p k t -> p (k t)").bitcast(F32)[:, 0:NTI * E].rearrange("p (i e) -> p i e", i=NTI)
        actf = actT[:].rearrange("p k t -> p (k t)").bitcast(F32)
        comb = K.sb("comb", [128, NTI, E], F32)
        chc = K.chan("cmb")
        t_cb = K.dma('sp', chc, comb[:], T['comb_all'].rearrange("(i p) e -> p i e", p=128))
        esel = K.sb("esel", [128, EL, E], F32)
        t_es = K.dma('sp', chc, esel[:], T['esel'])
        tokid = K.sb("tokid", [128, NTI, 16], I32)
        t_tk = K.dma('sp', chc, tokid[:], T['tokid16'])
        lstr = K.sb("lstr", [128, 128], F32)
        t_ls = K.dma('sp', chc, lstr[:], T['lstrict'])
        t_in = chc.tok()
        mm = K.sb("mymask", [128, NTI, EL], F32)
        t_m = None
        for el in range(EL):
            t1 = K.op('dve', lambda e, el=el: e.tensor_tensor(out=tmpm, in0=comb[:], in1=esel[:, el, :].unsqueeze(1).to_broadcast([128, NTI, E]), op=ALU.mult),
                      deps=[t_in, t_m])
            t_m = K.op('dve', lambda e, el=el: e.tensor_reduce(out=mm[:, :, el], in_=tmpm, axis=AX.X, op=ALU.add), deps=[t1])
        t_m = K.op('dve', lambda e: e.tensor_scalar(out=mm[:], in0=mm[:], scalar1=0.0, scalar2=None, op0=ALU.is_gt), deps=[t_m])
        cum = K.sb("cum", [128, NTI, EL], F32)
        t_c = K.op('dve', lambda e: e.memset(cum[:, 0, :], 0.0))
        for i in range(1, NTI):
            t_c = K.op('dve', lambda e, i=i: e.tensor_tensor(out=cum[:, i, :], in0=cum[:, i - 1, :], in1=mm[:, i - 1, :], op=ALU.add), deps=[t_c, t_m])
        ps_r = BK.f32(0)[:, 0:NTI * EL]
        K.op('pe', lambda e: e.matmul(ps_r, lhsT=lstr[:], rhs=mm[:].rearrange("p i e -> p (i e)"), start=True, stop=False), deps=[t_m, t_in], mark=False)
        t_r = K.op('pe', lambda e: e.matmul(ps_r, lhsT=C['ones32'][:], rhs=cum[:].rearrange("p i e -> p (i e)"), start=False, stop=True),
                   deps=[t_c, C['t_ones32']])
        rank_sb = K.sb("rank_sb", [128, NTI, EL], F32)
        t_rk = K.op('dve', lambda e: e.tensor_copy(out=rank_sb[:], in_=ps_r.rearrange("p (i e) -> p i e", i=NTI)), deps=[t_r])
        BK.free[0] = t_rk
        elcap = K.sb("elcap", [128, EL], F32)
        t_ec = K.dma('sp', chc, elcap[:], T['elcap4'])
        slotf = K.sb("slotf", [128, NTI, EL], F32)
        t_s = K.op('dve', lambda e: e.tensor_scalar(out=slotf[:], in0=rank_sb[:], scalar1=float(CAP), scalar2=None, op0=ALU.is_lt), deps=[t_rk])
        t_s = K.op('dve', lambda e: e.tensor_tensor(out=slotf[:], in0=slotf[:], in1=mm[:], op=ALU.mult), deps=[t_s, t_m])
        t_s2 = K.op('dve', lambda e: e.tensor_tensor(out=rank_sb[:], in0=rank_sb[:], in1=elcap[:].unsqueeze(1).to_broadcast([128, NTI, EL]), op=ALU.add), deps=[t_s, t_ec])
        t_s2 = K.op('dve', lambda e: e.tensor_scalar(out=rank_sb[:], in0=rank_sb[:], scalar1=-1.0e6, scalar2=None, op0=ALU.add), deps=[t_s2])
        t_s = K.op('dve', lambda e: e.tensor_tensor(out=slotf[:], in0=slotf[:], in1=rank_sb[:], op=ALU.mult), deps=[t_s2])
        t_s = K.op('dve', lambda e: e.tensor_scalar(out=slotf[:], in0=slotf[:], scalar1=1.0e6, scalar2=None, op0=ALU.add), deps=[t_s])
        sloti = K.sb("sloti", [128, EL, NTI], I32)
        t_si = K.op('dve', lambda e: e.tensor_copy(out=sloti[:].rearrange("p e i -> p i e"), in_=slotf[:]), deps=[t_s])
        zer = actf[:, 4096:5120].bitcast(I32)
        t_z = K.op('dve', lambda e: e.memset(zer, 0))
        chz = K.chan("z")
        t_zf = K.dma('sp', chz, T['idxbuf'].rearrange("(p j) o -> p (j o)", p=128), zer, deps=[t_z])
        chs = [K.chan("sc") for _ in range(4)]
        for el in range(EL):
            for i in range(NTI):
                c = chs[(el * NTI + i) % 4]
                K.dma('pool', c, T['idxbuf'], tokid[:, i, :], deps=[t_si, t_tk, t_zf],
                      indirect=dict(out_offset=bass.IndirectOffsetOnAxis(ap=sloti[:, el, i:i + 1], axis=0), in_offset=None,
                                    bounds_check=EL * CAP - 1, oob_is_err=False))
        t_scat = [c.tok() for c in chs]
        NPASS = CAP // 1024
        ring = WRing(K, "wring", 4, [KC, 512])
        xg = K.sb("xg", [128, 2, D], BF16)
        idxs = K.sb("idxs", [128, 8, 16], I32)
        chi = K.chan("idx")
        chg = [K.chan("xg"), K.chan("xg")]
        xgfree = [None, None]
        bgu = K.sb("bgu", [128, EL, 32], F32)
        t_bg = featmajor_rows(K, C, BK, 7, T['b_gu_loc'][layer], EL, 32, bgu[:].rearrange("p e c -> p c e"), rw=actf[:, 0:4096])
        bd = K.sb("bd", [1, D], BF16)
        chb = K.chan("bd")
        bdfree = None
        onesb = K.sb("onesb", [1, 128], BF16)
        t_ob = K.op('dve', lambda e: e.memset(onesb[:], 1.0))
        gS = K.sb("gS", [128, 2, 512], F32)
        sS = K.sb("sS", [128, 2, 512], F32)
        uS = K.sb("uS", [128, 2, 512], F32)
        ysb = K.sb("ysb", [128, 2, D], BF16)
        chy = [K.chan("y"), K.chan("y")]
        yfree = [None, None]
        gfree = [None, None]
        ufree = [None, None]
        XTfree = [t_m]
        actfree = [t_bg, t_zf]
        idxfree = []
        wgu = T['w_gu_loc']
        wd = T['w_down_loc']
        yi = 0
        for el in range(EL):
            t_bd = K.dma('pool', chb, bd[:], T['b_down_loc'][layer, el:el + 1, :], deps=[bdfree])
            for ps_ in range(NPASS):
                slot0 = el * CAP + ps_ * 1024
                t_ix = K.dma('sp', chi, idxs[:], T['idxbuf'][slot0:slot0 + 1024, :].rearrange("(s p) o -> p s o", p=128), deps=t_scat + idxfree)
                idxfree = []
                t_XT = []
                for st in range(8):
                    s = st % 2
                    tg = K.dma('pool', chg[s], xg[:, s, :], T['h_all'], deps=[t_ix, xgfree[s]],
                               indirect=dict(out_offset=None, in_offset=bass.IndirectOffsetOnAxis(ap=idxs[:, st, 0:1], axis=0)))
                    idxfree.append(tg)
                    for half in range(2):
                        b = half
                        psb = BK.bf16(b).rearrange("p (a c) -> p a c", a=8)
                        tp = None
                        for j in range(8):
                            kc = half * 8 + j
                            tp = K.op('pe', lambda e, kc=kc, j=j, s=s, psb=psb: e.transpose(out=psb[:, j, :], in_=xg[:, s, kc * 128:(kc + 1) * 128], identity=C['identb'][:]),
                                      deps=[tg, C['t_identb'], BK.free[b]], mark=(j == 7))
                        if half == 0:
                            tcp = K.op('act', lambda e, st=st, half=half, psb=psb: e.activation(out=XT[:, half * 8:half * 8 + 8, st * 128:(st + 1) * 128], in_=psb, func=AF.Copy),
                                       deps=[tp] + XTfree)
                        else:
                            tcp = K.op('dve', lambda e, st=st, half=half, psb=psb: e.tensor_copy(out=XT[:, half * 8:half * 8 + 8, st * 128:(st + 1) * 128], in_=psb),
                                       deps=[tp] + XTfree)
                        BK.free[b] = tcp
                        t_XT.append(tcp)
                    xgfree[s] = tp
                XTfree = []
                lastgu = None
                t_act = []
                for i in range(4):
                    sg, Wg, tWg = ring.load(wpiece(wgu[layer, el], 512 * i, 512))
                    su, Wu, tWu = ring.load(wpiece(wgu[layer, el], 2048 + 512 * i, 512))
                    for j in range(4):
                        fc = 4 * i + j
                        for c2 in range(2):
                            bg, bu = 2 + c2 * 2, 3 + c2 * 2
                            cols = slice(c2 * 512, (c2 + 1) * 512)
                            for kc in range(KC):
                                tG = K.op('pe', lambda e, kc=kc, j=j, Wg=Wg, bg=bg, cols=cols: e.matmul(
                                    BK.f32(bg), lhsT=Wg[:, kc, j * 128:(j + 1) * 128], rhs=XT[:, kc, cols], start=(kc == 0), stop=(kc == KC - 1)),
                                    deps=[tWg, BK.free[bg]] + t_XT, mark=(kc == KC - 1))
                            for kc in range(KC):
                                tU = K.op('pe', lambda e, kc=kc, j=j, Wu=Wu, bu=bu, cols=cols: e.matmul(
                                    BK.f32(bu), lhsT=Wu[:, kc, j * 128:(j + 1) * 128], rhs=XT[:, kc, cols], start=(kc == 0), stop=(kc == KC - 1)),
                                    deps=[tWu, BK.free[bu]], mark=(kc == KC - 1))
                            lastgu = tU
                            t1 = K.op('dve', lambda e, fc=fc, bg=bg, c2=c2, el=el: e.tensor_scalar(out=gS[:, c2, :], in0=BK.f32(bg), scalar1=bgu[:, el, fc:fc + 1], scalar2=7.0,
                                                                                                op0=ALU.add, op1=ALU.min), deps=[tG, t_bg, gfree[c2]])
                            BK.free[bg] = t1
                            t2 = K.op('act', lambda e, c2=c2: e.activation(out=sS[:, c2, :], in_=gS[:, c2, :], func=AF.Sigmoid, scale=1.702), deps=[t1])
                            t3 = K.op('dve', lambda e, fc=fc, bu=bu, c2=c2, el=el: e.tensor_scalar(out=uS[:, c2, :], in0=BK.f32(bu), scalar1=bgu[:, el, 16 + fc:17 + fc], scalar2=7.0,
                                                                                                op0=ALU.add, op1=ALU.min), deps=[tU, ufree[c2]])
                            BK.free[bu] = t3
                            t4 = K.op('dve', lambda e, c2=c2: e.tensor_scalar(out=uS[:, c2, :], in0=uS[:, c2, :], scalar1=-7.0, scalar2=1.0, op0=ALU.max, op1=ALU.add), deps=[t3])
                            t5 = K.op('dve', lambda e, c2=c2: e.tensor_tensor(out=gS[:, c2, :], in0=gS[:, c2, :], in1=sS[:, c2, :], op=ALU.mult), deps=[t2, t1])
                            t6 = K.op('dve', lambda e, c2=c2, fc=fc, cols=cols: e.tensor_tensor(out=actT[:, fc, cols], in0=gS[:, c2, :], in1=uS[:, c2, :], op=ALU.mult),
                                      deps=[t5, t4] + actfree)
                            gfree[c2] = t6
                            ufree[c2] = t6
                            t_act.append(t6)
                    ring.release(sg, lastgu)
                    ring.release(su, lastgu)
                actfree = []
                XTfree = [lastgu]
                Wds = []
                for dcg in range(4):
                    Wds.append(ring.load(wpiece(wd[layer, el], 512 * dcg, 512)))
                lastd = None
                for st in range(8):
                    ys = st % 2
                    tcps = []
                    for dcg in range(4):
                        sd, Wd, tWd = Wds[dcg]
                        bk = 6 + (st * 4 + dcg) % 2
                        for kc in range(KC):
                            K.op('pe', lambda e, kc=kc, st=st, Wd=Wd, bk=bk: e.matmul(BK.f32(bk), lhsT=actT[:, kc, st * 128:(st + 1) * 128], rhs=Wd[:, kc, :],
                                                                                 start=(kc == 0), stop=False), deps=[tWd, BK.free[bk]] + (t_act if kc == 0 else []), mark=False)
                        lastd = K.op('pe', lambda e, bk=bk, dcg=dcg, el=el: e.matmul(BK.f32(bk), lhsT=onesb[0:1, :], rhs=bd[0:1, dcg * 512:(dcg + 1) * 512],
                                                                               start=False, stop=True), deps=[t_bd, t_ob])
                        if dcg % 2 == 0:
                            tcp = K.op('act', lambda e, ys=ys, dcg=dcg, bk=bk: e.activation(out=ysb[:, ys, dcg * 512:(dcg + 1) * 512], in_=BK.f32(bk), func=AF.Copy),
                                       deps=[lastd, yfree[ys]])
                        else:
                            tcp = K.op('dve', lambda e, ys=ys, dcg=dcg, bk=bk: e.tensor_copy(out=ysb[:, ys, dcg * 512:(dcg + 1) * 512], in_=BK.f32(bk)),
                                       deps=[lastd, yfree[ys]])
                        BK.free[bk] = tcp
                        tcps.append(tcp)
                    yfree[ys] = K.dma('sp', chy[ys], T['ybuf'][slot0 + st * 128: slot0 + (st + 1) * 128, :], ysb[:, ys, :], deps=tcps)
                for (sd, Wd, tWd) in Wds:
                    ring.release(sd, lastd)
                actfree = [lastd]
                bdfree = lastd
        K.flush(final_deps=[chy[0].tok(), chy[1].tok()])


def reload_xT(K, T, xT):
    ch = K.chan("xr")
    return K.dma('sp', ch, xT[:], T['xT_in'].rearrange("p (k t) -> p k t", k=KC))


def rope_tables(K, T, cosT, sinT, tmpA, tmpI, ti, deps=()):
    ch = K.chan("pos")
    posi = tmpI
    t_p = K.dma('sp', ch, posi, T['pos_loc'].to_broadcast([128, NT]), deps=list(deps))
    invf = K.sb("invf", [128, 1], F32)
    t_i = K.dma('sp', ch, invf[:], T['invf'])
    t_all = ch.tok()
    a = tmpA
    INV2PI = 1.0 / (2.0 * np.pi)

    def red(dst, phase, dep):
        t = K.op('dve', lambda e: e.tensor_scalar(out=dst, in0=a, scalar1=INV2PI, scalar2=phase, op0=ALU.mult, op1=ALU.add), deps=dep)
        t = K.op('dve', lambda e: e.tensor_copy(out=posi, in_=dst), deps=[t])
        t = K.op('dve', lambda e: e.tensor_copy(out=ti, in_=posi), deps=[t])
        t = K.op('dve', lambda e: e.tensor_tensor(out=dst, in0=dst, in1=ti, op=ALU.subtract), deps=[t])
        t = K.op('dve', lambda e: e.tensor_scalar(out=ti, in0=dst, scalar1=0.5, scalar2=None, op0=ALU.is_gt), deps=[t])
        t = K.op('dve', lambda e: e.tensor_tensor(out=dst, in0=dst, in1=ti, op=ALU.subtract), deps=[t])
        t = K.op('dve', lambda e: e.tensor_scalar(out=ti, in0=dst, scalar1=-0.5, scalar2=None, op0=ALU.is_lt), deps=[t])
        t = K.op('dve', lambda e: e.tensor_tensor(out=dst, in0=dst, in1=ti, op=ALU.add), deps=[t])
        t = K.op('act', lambda e: e.activation(out=dst, in_=dst, func=AF.Sin, scale=2.0 * np.pi), deps=[t])
        return t
    t0 = K.op('dve', lambda e: e.tensor_copy(out=a, in_=posi), deps=[t_all])
    t0 = K.op('dve', lambda e: e.tensor_scalar(out=a, in0=a, scalar1=invf[:, 0:1], scalar2=None, op0=ALU.mult), deps=[t0])
    ts = red(sinT, 0.0, [t0])
    tc = red(cosT, 0.25, [ts])
    return tc


def combine(K, C, BK, T, xT, adaT, layer, t_x, scr, ca=None):
    NTI = 64
    if ca is None:
        ca = K.sb("ca", [128, NTI, E], F32)[:]
    ch = K.chan("cmb")
    K.dma('sp', ch, ca, T['comb_all'].rearrange("(i p) e -> p i e", p=128))
    psel = K.sb("psel", [128, NTI], F32)
    K.dma('sp', ch, psel[:], T['psel'])
    cl = K.sb("cl", [128, 8, E], F32)
    K.dma('sp', ch, cl[:], T['comb_loc'].rearrange("(t p) e -> p t e", p=128))
    lstr = K.sb("lstr", [128, 128], F32)
    K.dma('sp', ch, lstr[:], T['lstrict'])
    ecap = K.sb("ecap", [128, E], F32)
    K.dma('sp', ch, ecap[:], T['ecap'])
    t_in = ch.tok()
    t1 = K.op('dve', lambda e: e.tensor_scalar(out=ca, in0=ca, scalar1=0.0, scalar2=None, op0=ALU.is_gt), deps=[t_in])
    t1 = K.op('dve', lambda e: e.tensor_tensor(out=ca, in0=ca, in1=psel[:].unsqueeze(2).to_broadcast([128, NTI, E]), op=ALU.mult), deps=[t1])
    cum = K.sb("cumL", [128, 8, E], F32)
    t_b = K.op('dve', lambda e: e.tensor_reduce(out=cum[:, 0, :], in_=ca.rearrange("p i e -> p e i"), axis=AX.X, op=ALU.add), deps=[t1])
    ml = K.sb("ml", [128, 8, E], F32)
    t_ml = K.op('dve', lambda e: e.tensor_scalar(out=ml[:], in0=cl[:], scalar1=0.0, scalar2=None, op0=ALU.is_gt), deps=[t_in])
    t_c = t_b
    for tt in range(1, 8):
        t_c = K.op('dve', lambda e, tt=tt: e.tensor_tensor(out=cum[:, tt, :], in0=cum[:, tt - 1, :], in1=ml[:, tt - 1, :], op=ALU.add), deps=[t_c, t_ml])
    ps_r = BK.f32(0)[:, 0:8 * E]
    K.op('pe', lambda e: e.matmul(ps_r, lhsT=lstr[:], rhs=ml[:].rearrange("p t e -> p (t e)"), start=True, stop=False), deps=[t_ml, BK.free[0]], mark=False)
    t_r = K.op('pe', lambda e: e.matmul(ps_r, lhsT=C['ones32'][:], rhs=cum[:].rearrange("p t e -> p (t e)"), start=False, stop=True), deps=[t_c, C['t_ones32']])
    key = K.sb("key", [128, 8, E], F32)
    t_k = K.op('dve', lambda e: e.tensor_tensor(out=key[:], in0=ps_r.rearrange("p (t e) -> p t e", t=8), in1=ecap[:].unsqueeze(1).to_broadcast([128, 8, E]), op=ALU.add),
               deps=[t_r, t_in])
    BK.free[0] = t_k
    top8 = K.sb("ctop8", [128, 8, 8], F32)
    t8 = None
    for tt in range(8):
        t8 = K.op('dve', lambda e, tt=tt: e.max(out=top8[:, tt, :], in_=cl[:, tt, :]), deps=[t_in])
    rowf = K.sb("rowf", [128, 8, 4], F32)
    mk = K.sb("mk", [128, E], F32)
    t_rw = None
    for tt in range(8):
        for k in range(4):
            t1 = K.op('dve', lambda e, tt=tt, k=k: e.tensor_scalar(out=mk[:], in0=cl[:, tt, :], scalar1=top8[:, tt, k:k + 1], scalar2=None, op0=ALU.is_equal), deps=[t8, t_rw])
            t1 = K.op('dve', lambda e, tt=tt: e.tensor_tensor(out=mk[:], in0=mk[:], in1=key[:, tt, :], op=ALU.mult), deps=[t1, t_k])
            t_rw = K.op('dve', lambda e, tt=tt, k=k: e.tensor_reduce(out=rowf[:, tt, k:k + 1], in_=mk[:], axis=AX.X, op=ALU.add), deps=[t1])
    rowi = K.sb("rowi", [128, 8, 4], I32)
    t_ri = K.op('dve', lambda e: e.tensor_copy(out=rowi[:], in_=rowf[:]), deps=[t_rw])
    yk = scr[:, 0:4096].bitcast(BF16).rearrange("p (k d) -> p k d", k=4)
    acc = scr[:, 4096:6144]
    chy = [K.chan("yk") for _ in range(4)]
    ykfree = [None] * 4
    accfree = None
    t_xo = t_x
    cmbtmp = [K.sb("cmbtmp0", [128, 4, 128], F32), K.sb("cmbtmp1", [128, 4, 128], F32)]
    for tt in range(8):
        tg = []
        for k in range(4):
            tg.append(K.dma('pool', chy[k], yk[:, k, :], T['yall'], deps=[t_ri, ykfree[k]],
                            indirect=dict(out_offset=None, in_offset=bass.IndirectOffsetOnAxis(ap=rowi[:, tt, k:k + 1], axis=0))))
        ta = K.op('dve', lambda e, tt=tt: e.tensor_scalar(out=acc, in0=yk[:, 0, :], scalar1=top8[:, tt, 0:1], scalar2=None, op0=ALU.mult), deps=[tg[0], accfree, t8])
        ykfree[0] = ta
        for k in range(1, 4):
            ta = K.op('dve', lambda e, tt=tt, k=k: e.scalar_tensor_tensor(out=acc, in0=yk[:, k, :], scalar=top8[:, tt, k:k + 1], in1=acc, op0=ALU.mult, op1=ALU.add), deps=[tg[k], ta])
            ykfree[k] = ta
        tp = None
        for g in range(4):
            b = 1 + (g % 2)
            ps = BK.f32(b).rearrange("p (a c) -> p a c", a=4)
            for j in range(4):
                dc = 4 * g + j
                tp = K.op('pe', lambda e, dc=dc, j=j, ps=ps: e.transpose(out=ps[:, j, :], in_=acc[:, dc * 128:(dc + 1) * 128], identity=C['ident32'][:]),
                          deps=[ta, C['t_ident32'], BK.free[b]], mark=(j == 3))
            tmpg = cmbtmp[g % 2]
            t1 = K.op('dve', lambda e, g=g, ps=ps, tmpg=tmpg: e.tensor_tensor(out=tmpg[:], in0=ps, in1=adaT[:, layer, 80 + 4 * g:84 + 4 * g].unsqueeze(2).to_broadcast([128, 4, 128]), op=ALU.mult),
                       deps=[tp, t_xo])
            BK.free[b] = t1
            t_xo = K.op('dve', lambda e, g=g, tt=tt, tmpg=tmpg: e.tensor_tensor(out=xT[:, 4 * g:4 * g + 4, tt * 128:(tt + 1) * 128], in0=xT[:, 4 * g:4 * g + 4, tt * 128:(tt + 1) * 128],
                                                                          in1=tmpg[:], op=ALU.add), deps=[t1])
        accfree = tp
    return t_xo


def phase3(nc, T):
    with ExitStack() as es:
        K = Ctx(nc, es)
        C = load_consts(K, T)
        BK = Banks(K, 8)
        xT = K.sb("xT", [128, KC, NT], F32)
        adaT = K.sb("adaT", [128, 2, 96], F32)
        featT = K.sb("featT", [128, KC, 15], F32)
        scr = K.sb("scr", [128, 6144], F32)
        hk = K.sb("hk", [128, KC, NT], BF16)
        t_feat = featmajor_rows(K, C, BK, 7, T['smallpack'], 15, KC, featT[:], rw=scr[:, 0:2048])
        t_ada = load_ada(K, C, BK, 6, T, adaT)
        t_x = reload_xT(K, T, xT)
        K.wait('pool', [t_feat])
        K.wait('dve', [t_ada])
        ca_ap = hk[:].rearrange("p k t -> p (k t)").bitcast(F32)[:, 0:64 * E].rearrange("p (i e) -> p i e", i=64)
        t_x = combine(K, C, BK, T, xT, adaT, 0, [t_x, t_ada], scr[:], ca=ca_ap)
        chs = K.chan("sp")
        t_sp = K.dma('sp', chs, T['xT_spill'].rearrange("p (k t) -> p k t", k=KC), xT[:], deps=[t_x])
        rstd = K.sb("rstd", [128, NT], F32)
        sq = K.sb("sq", [128, 2, NT], F32)
        t_r = rms_rstd(K, C, BK, [0, 1], xT, NT, rstd, sq, [t_x])
        t_h = None
        for kc in range(KC):
            t_h = K.op('dve', lambda e, kc=kc: e.scalar_tensor_tensor(out=hk[:, kc, :], in0=xT[:, kc, :], scalar=featT[:, kc, 7:8], in1=rstd[:], op0=ALU.mult, op1=ALU.mult),
                       deps=[t_r, t_feat])
        cosT, sinT = sq[:, 0, :], sq[:, 1, :]
        t_rope = rope_tables(K, T, cosT, sinT, scr[:, 0:1024], scr[:, 1024:2048].bitcast(I32), scr[:, 4096:5120], deps=[t_x, t_r, t_h])
        protf = K.sb("protf", [128, 128], F32)
        chp = K.chan("prot")
        t_pf = K.dma('sp', chp, protf[:], T['protT'])
        protb = K.sb("protb", [128, 128], BF16)
        t_pb = K.op('dve', lambda e: e.tensor_copy(out=protb[:], in_=protf[:]), deps=[t_pf])
        ring = WRing(K, "wring", 3, [KC, 512])
        k32 = scr[:, 2048:3072]
        kb = scr[:, 3072:3584].bitcast(BF16)
        kro = scr[:, 3584:4096].bitcast(BF16)
        pk = scr[:, 5120:6144]
        km = K.sb("km", [128, NH, 4], F32)
        chk = K.chan("ko")
        wkv = T['w_kv']
        k32free = []
        kbfree = None
        krofree = None
        t_kdone = []
        for hg in range(4):
            s_, Wk, tW = ring.load(wpiece(wkv, 512 * hg, 512))
            lastpe = None
            for j in range(4):
                h = 4 * hg + j
                t6s, t7s = [], []
                for half in range(2):
                    bk = 2 + half
                    cols = slice(half * 512, (half + 1) * 512)
                    for kc in range(KC):
                        lastpe = K.op('pe', lambda e, kc=kc, j=j, Wk=Wk, bk=bk, cols=cols: e.matmul(
                            BK.f32(bk), lhsT=Wk[:, kc, j * 128:(j + 1) * 128], rhs=hk[:, kc, cols], start=(kc == 0), stop=(kc == KC - 1)),
                            deps=[tW, t_h, BK.free[bk]], mark=(kc == KC - 1))
                    t1 = K.op('act', lambda e, cols=cols, bk=bk: e.activation(out=k32[:, cols], in_=BK.f32(bk), func=AF.Copy), deps=[lastpe, t_rope] + k32free)
                    BK.free[bk] = t1
                    t2 = K.op('dve', lambda e, cols=cols: e.tensor_copy(out=kb[:, cols], in_=k32[:, cols]), deps=[t1, kbfree])
                    tp = K.op('pe', lambda e, half=half, cols=cols: e.matmul(BK.f32(4 + half), lhsT=protb[:], rhs=kb[:, cols], start=True, stop=True),
                              deps=[t2, t_pb, BK.free[4 + half]])
                    t3 = K.op('dve', lambda e, cols=cols: e.tensor_tensor(out=k32[:, cols], in0=k32[:, cols], in1=cosT[:, cols], op=ALU.mult), deps=[t2, t_rope])
                    t4 = K.op('dve', lambda e, half=half, cols=cols: e.tensor_tensor(out=pk[:, cols], in0=BK.f32(4 + half), in1=sinT[:, cols], op=ALU.mult), deps=[tp])
                    BK.free[4 + half] = t4
                    t5 = K.op('dve', lambda e, cols=cols: e.tensor_tensor(out=k32[:, cols], in0=k32[:, cols], in1=pk[:, cols], op=ALU.add), deps=[t3, t4])
                    kbfree = tp
                    t6 = K.op('act', lambda e, cols=cols: e.activation(out=kro[:, cols], in_=k32[:, cols], func=AF.Copy), deps=[t5, krofree])
                    t7 = K.op('dve', lambda e, half=half, h=h, cols=cols: e.tensor_reduce(out=km[:, h, 2 * half:2 * half + 2], in_=k32[:, cols].rearrange("p (b t) -> p b t", b=2),
                                                                                       axis=AX.X, op=ALU.add), deps=[t5])
                    t6s.append(t6)
                    t7s.append(t7)
                k32free = t6s + t7s
                krofree = K.dma('sp', chk, T['kT_loc'][h], kro, deps=t6s)
            ring.release(s_, lastpe)
        t_kdone = k32free
        t_km = K.op('dve', lambda e: e.tensor_scalar(out=km[:], in0=km[:], scalar1=1.0 / 256, scalar2=None, op0=ALU.mult), deps=t_kdone)
        chm = K.chan("km")
        K.dma('sp', chm, T['kmean_loc'], km[:], deps=[t_km])
        vst = scr[:, 4096:6144].bitcast(BF16).rearrange("p (t c) -> p t c", t=8)
        chv = K.chan("vo")
        vfree = None
        for n in range(4):
            s_, Wv, tW = ring.load(wpiece(wkv, 2048 + 512 * n, 512))
            lastpe = None
            for tt in range(8):
                bk = 2 + tt % 4
                for kc in range(KC):
                    lastpe = K.op('pe', lambda e, kc=kc, tt=tt, Wv=Wv, bk=bk: e.matmul(BK.f32(bk), lhsT=hk[:, kc, tt * 128:(tt + 1) * 128], rhs=Wv[:, kc, :],
                                                                                 start=(kc == 0), stop=(kc == KC - 1)), deps=[tW, t_h, BK.free[bk]], mark=(kc == KC - 1))
                if tt % 2 == 0:
                    tcp = K.op('act', lambda e, tt=tt, bk=bk: e.activation(out=vst[:, tt, :], in_=BK.f32(bk), func=AF.Copy), deps=[lastpe, vfree, t_km])
                else:
                    tcp = K.op('dve', lambda e, tt=tt, bk=bk: e.tensor_copy(out=vst[:, tt, :], in_=BK.f32(bk)), deps=[lastpe, vfree, t_km])
                BK.free[bk] = tcp
                if tt == 6:
                    tcp6 = tcp
            ring.release(s_, lastpe)
            vfree = K.dma('sp', chv, T['v_loc'][:, n * 512:(n + 1) * 512].rearrange("(t p) c -> p t c", p=128), vst, deps=[tcp, tcp6])
        K.flush(final_deps=[chs.tok(), chk.tok(), chm.tok(), chv.tok()])


def phase6(nc, T):
    with ExitStack() as es:
        K = Ctx(nc, es)
        C = load_consts(K, T)
        BK = Banks(K, 8)
        xT = K.sb("xT", [128, KC, NT], F32)
        adaT = K.sb("adaT", [128, 2, 96], F32)
        featT = K.sb("featT", [128, KC, 15], F32)
        scr = K.sb("scr", [128, 6144], F32)
        t_feat = featmajor_rows(K, C, BK, 7, T['smallpack'], 15, KC, featT[:], rw=scr[:, 0:2048])
        t_ada = load_ada(K, C, BK, 6, T, adaT)
        t_x = reload_xT(K, T, xT)
        K.wait('dve', [t_ada])
        K.wait('pool', [t_feat])
        t_x = combine(K, C, BK, T, xT, adaT, 1, [t_x, t_ada], scr[:])
        rstd = K.sb("rstd", [128, NT], F32)
        sq = K.sb("sq", [128, 2, NT], F32)
        t_r = rms_rstd(K, C, BK, [0, 1], xT, NT, rstd, sq, [t_x])
        t_o = None
        for kc in range(KC):
            t_o = K.op('dve', lambda e, kc=kc: e.scalar_tensor_tensor(out=xT[:, kc, :], in0=xT[:, kc, :], scalar=featT[:, kc, 8:9], in1=rstd[:], op0=ALU.mult, op1=ALU.mult),
                       deps=[t_r, t_feat])
        orow = scr[:, 0:4096].rearrange("p (s d) -> p s d", s=2)
        cho = [K.chan("o"), K.chan("o")]
        ofree = [None, None]
        for tt in range(8):
            s = tt % 2
            tcps = []
            for g in range(4):
                b = 2 + g
                ps = BK.f32(b).rearrange("p (a c) -> p a c", a=4)
                tp = None
                for j in range(4):
                    kc = 4 * g + j
                    tp = K.op('pe', lambda e, kc=kc, j=j, tt=tt, ps=ps: e.transpose(out=ps[:, j, :], in_=xT[:, kc, tt * 128:(tt + 1) * 128], identity=C['ident32'][:]),
                              deps=[t_o, C['t_ident32'], BK.free[b]], mark=(j == 3))
                if g % 2 == 0:
                    tcp = K.op('act', lambda e, g=g, s=s, b=b: e.activation(out=orow[:, s, g * 512:(g + 1) * 512], in_=BK.f32(b), func=AF.Copy), deps=[tp, ofree[s]])
                else:
                    tcp = K.op('dve', lambda e, g=g, s=s, b=b: e.tensor_copy(out=orow[:, s, g * 512:(g + 1) * 512], in_=BK.f32(b)), deps=[tp, ofree[s]])
                BK.free[b] = tcp
                tcps.append(tcp)
            ofree[s] = K.dma('sp', cho[s], T['out_loc'][tt * 128:(tt + 1) * 128, :], orow[:, s, :], deps=tcps)
        K.flush(final_deps=[cho[0].tok(), cho[1].tok()])


def phase4(nc, T):
    SCALE = DH ** -0.5
    with ExitStack() as es:
        K = Ctx(nc, es)
        C = load_consts(K, T)
        BK = Banks(K, 8)
        xT = K.sb("xT", [128, KC, NT], F32)
        xb = xT[:].rearrange("p k t -> p (k t)").bitcast(BF16)
        qT = xb[:, 0:16384].rearrange("p (h t) -> p h t", h=NH)
        otok = xb[:, 16384:32768].rearrange("p (t d) -> p t d", t=8)
        hT = K.sb("hT", [128, KC, NT], BF16)
        featT = K.sb("featT", [128, KC, 15], F32)
        adaT = K.sb("adaT", [128, 2, 96], F32)
        scr = K.sb("scr", [128, 8192], F32)
        t_feat = featmajor_rows(K, C, BK, 7, T['smallpack'], 15, KC, featT[:], rw=scr[:, 6144:8192])
        t_ada = load_ada(K, C, BK, 6, T, adaT)
        t_x = reload_xT(K, T, xT)
        sq = scr[:, 0:2048].rearrange("p (a b) -> p a b", a=2)
        rstd = scr[:, 4096:5120]
        gs1 = K.sb("gs1", [128, KC], F32)
        t_g = K.op('dve', lambda e: e.scalar_tensor_tensor(out=gs1[:], in0=adaT[:, 1, 16:32], scalar=1.0, in1=featT[:, :, 2], op0=ALU.add, op1=ALU.mult), deps=[t_ada, t_feat])
        t_r = rms_rstd(K, C, BK, [0, 1], xT, NT, rstd, sq, [t_x])
        tmp = scr[:, 2048:4096].rearrange("p (a b) -> p a b", a=2)
        tmpfree = [None, None]
        t_h = None
        for kc in range(KC):
            s = kc % 2
            t1 = K.op('dve', lambda e, kc=kc, s=s: e.tensor_tensor(out=tmp[:, s, :], in0=xT[:, kc, :], in1=rstd, op=ALU.mult), deps=[t_r, tmpfree[s]])
            t_h = K.op('act', lambda e, kc=kc, s=s: e.activation(out=hT[:, kc, :], in_=tmp[:, s, :], func=AF.Identity, scale=gs1[:, kc:kc + 1], bias=adaT[:, 1, kc:kc + 1]), deps=[t1, t_g])
            tmpfree[s] = t_h
        cs = K.sb("cs", [128, 2, NT], F32)
        cosT, sinT = cs[:, 0, :], cs[:, 1, :]
        t_rope = rope_tables(K, T, cosT, sinT, scr[:, 0:1024], scr[:, 1024:2048].bitcast(I32), scr[:, 5120:6144], deps=[t_h])
        protf = K.sb("protf", [128, 128], F32)
        chp = K.chan("prot")
        t_pf = K.dma('sp', chp, protf[:], T['protT'])
        protb = K.sb("protb", [128, 128], BF16)
        t_pb = K.op('dve', lambda e: e.tensor_copy(out=protb[:], in_=protf[:]), deps=[t_pf])
        kmT = K.sb("kmT", [128, NH, 8], F32)
        chm = K.chan("km")
        t_km = K.dma('sp', chm, kmT[:], T['kmean_all'].rearrange("h p n -> p h n"))
        gate = K.sb("gate", [128, 8, NH, 8], F32)
        ring = WRing(K, "wring", 3, [KC, 256])
        q32 = scr[:, 2048:3072]
        qb = scr[:, 3072:3584].bitcast(BF16)
        pq = scr[:, 6144:7168]
        wq = T['attn_w_q']
        q32free = []
        qbfree = None
        t_q = None
        for hg in range(8):
            s_, Wq, tW = ring.load(wpiece(wq, 256 * hg, 256))
            lastpe = None
            for j in range(2):
                h = 2 * hg + j
                t5s = []
                for half in range(2):
                    bk = 2 + half
                    cols = slice(half * 512, (half + 1) * 512)
                    for kc in range(KC):
                        lastpe = K.op('pe', lambda e, kc=kc, j=j, Wq=Wq, bk=bk, cols=cols: e.matmul(
                            BK.f32(bk), lhsT=Wq[:, kc, j * 128:(j + 1) * 128], rhs=hT[:, kc, cols], start=(kc == 0), stop=(kc == KC - 1)),
                            deps=[tW, t_h, BK.free[bk]], mark=(kc == KC - 1))
                    t1 = K.op('act', lambda e, cols=cols, bk=bk: e.activation(out=q32[:, cols], in_=BK.f32(bk), func=AF.Copy), deps=[lastpe, t_r, t_h] + q32free)
                    BK.free[bk] = t1
                    t2 = K.op('dve', lambda e, cols=cols: e.tensor_copy(out=qb[:, cols], in_=q32[:, cols]), deps=[t1, qbfree])
                    tp = K.op('pe', lambda e, half=half, cols=cols: e.matmul(BK.f32(4 + half), lhsT=protb[:], rhs=qb[:, cols], start=True, stop=True), deps=[t2, t_pb, BK.free[4 + half]])
                    t3 = K.op('dve', lambda e, cols=cols: e.tensor_tensor(out=q32[:, cols], in0=q32[:, cols], in1=cosT[:, cols], op=ALU.mult), deps=[t2, t_rope])
                    t4 = K.op('dve', lambda e, half=half, cols=cols: e.tensor_tensor(out=pq[:, cols], in0=BK.f32(4 + half), in1=sinT[:, cols], op=ALU.mult), deps=[tp])
                    BK.free[4 + half] = t4
                    t5 = K.op('dve', lambda e, cols=cols: e.tensor_tensor(out=q32[:, cols], in0=q32[:, cols], in1=pq[:, cols], op=ALU.add), deps=[t3, t4])
                    t_q = K.op('act', lambda e, cols=cols, h=h: e.activation(out=qT[:, h, cols], in_=q32[:, cols], func=AF.Copy), deps=[t5])
                    t5s.append(t5)
                    qbfree = tp
                psg = BK.f32(6)[:, 0:64].rearrange("p (t n) -> p t n", t=8)
                tg = None
                for qt in range(8):
                    tg = K.op('pe', lambda e, qt=qt, h=h, psg=psg: e.matmul(psg[:, qt, :], lhsT=q32[:, qt * 128:(qt + 1) * 128], rhs=kmT[:, h, :], start=True, stop=True),
                              deps=t5s + [t_km, BK.free[6]], mark=(qt == 7))
                tgc = K.op('dve', lambda e, h=h, psg=psg: e.tensor_copy(out=gate[:, :, h, :], in_=psg), deps=[tg])
                BK.free[6] = tgc
                q32free = [tg, t_q]
            ring.release(s_, lastpe)
        elig = K.sb("elig", [128, 8, 8], F32)
        own = K.sb("own", [128, 8, 8], F32)
        che = K.chan("el")
        K.dma('sp', che, elig[:], T['elig'].rearrange("(t p) n -> p t n", p=128))
        K.dma('sp', che, own[:], T['own'].rearrange("(t p) n -> p t n", p=128))
        t_el = che.tok()
        pen = K.sb("pen", [128, 8, 8], F32)
        t_pen = K.op('dve', lambda e: e.tensor_scalar(out=pen[:], in0=elig[:], scalar1=-1.0, scalar2=1.0e30, op0=ALU.add, op1=ALU.mult), deps=[t_el])
        eb = elig[:].unsqueeze(2).to_broadcast([128, 8, NH, 8])
        t1 = K.op('dve', lambda e: e.tensor_tensor(out=gate[:], in0=gate[:], in1=eb, op=ALU.mult), deps=[tgc, t_el])
        t1 = K.op('dve', lambda e: e.tensor_tensor(out=gate[:], in0=gate[:], in1=pen[:].unsqueeze(2).to_broadcast([128, 8, NH, 8]), op=ALU.add), deps=[t1, t_pen])
        top = K.sb("gtop", [128, 8, NH, 8], F32)
        tt_ = None
        for qt in range(8):
            for h in range(NH):
                tt_ = K.op('dve', lambda e, qt=qt, h=h: e.max(out=top[:, qt, h, :], in_=gate[:, qt, h, :]), deps=[t1])
        biasb = K.sb("biasb", [128, 8, NH, 8], F32)
        t2 = K.op('dve', lambda e: e.tensor_tensor(out=biasb[:], in0=gate[:], in1=top[:, :, :, 2:3].to_broadcast([128, 8, NH, 8]), op=ALU.is_ge), deps=[tt_])
        t2 = K.op('dve', lambda e: e.tensor_tensor(out=biasb[:], in0=biasb[:], in1=eb, op=ALU.mult), deps=[t2])
        t2 = K.op('dve', lambda e: e.tensor_tensor(out=biasb[:], in0=biasb[:], in1=own[:].unsqueeze(2).to_broadcast([128, 8, NH, 8]), op=ALU.add), deps=[t2])
        t_bias = K.op('dve', lambda e: e.tensor_scalar(out=biasb[:], in0=biasb[:], scalar1=-1.0, scalar2=1.0e30, op0=ALU.add, op1=ALU.mult), deps=[t2])
        kTh = K.sb("kTh", [128, 2048], BF16)
        Vh = K.sb("Vh", [128, 16, 128], BF16)
        cm = K.sb("cm", [128, 2048], BF16)
        chk, chv, chc = K.chan("kT"), K.chan("V"), K.chan("cm")
        sc = [scr[:, 0:2048], scr[:, 2048:4096]]
        Pb = [scr[:, 4096:5120].bitcast(BF16), scr[:, 5120:6144].bitcast(BF16)]
        PT = [scr[:, 6144:7168].bitcast(BF16).rearrange("p (k q) -> p k q", k=16), scr[:, 7168:8192].bitcast(BF16).rearrange("p (k q) -> p k q", k=16)]
        small = K.sb("asmall", [128, 2, 32], F32)
        kfree, vfree, cmfree = [], [], None
        scfree = [[], []]
        Pfree = [[], []]
        PTfree = [[], []]
        smfree = [[], []]
        it = 0
        t_o = None
        for h in range(NH):
            t_k = K.dma('sp', chk, kTh[:], T['kT_all'][h], deps=kfree)
            t_v = K.dma('sp', chv, Vh[:], T['v_all'][:, h * 128:(h + 1) * 128].rearrange("(k p) d -> p k d", p=128), deps=vfree)
            kfree, vfree = [], []
            for qt in range(8):
                s = it % 2
                it += 1
                t_cm = K.dma('sp', chc, cm[:], T['cmask'][qt * 128:(qt + 1) * 128, :], deps=[cmfree])
                tsc = []
                tS = None
                for c in range(4):
                    tS = K.op('pe', lambda e, c=c, h=h, qt=qt: e.matmul(BK.f32(c), lhsT=qT[:, h, qt * 128:(qt + 1) * 128], rhs=kTh[:, c * 512:(c + 1) * 512], start=True, stop=True),
                              deps=[t_k, t_q, BK.free[c]])
                    ta = K.op('dve', lambda e, c=c, s=s: e.tensor_tensor(out=sc[s][:, c * 512:(c + 1) * 512], in0=BK.f32(c), in1=cm[:, c * 512:(c + 1) * 512], op=ALU.add),
                              deps=[tS, t_cm] + scfree[s])
                    BK.free[c] = ta
                    tsc.append(ta)
                kfree.append(tS)
                cmfree = ta
                sm = small[:, s, :]
                tm = K.op('dve', lambda e, s=s, sm=sm: e.tensor_reduce(out=sm[:, 0:1], in_=sc[s], axis=AX.X, op=ALU.max), deps=tsc + smfree[s])
                tm = K.op('dve', lambda e, sm=sm: e.tensor_scalar(out=sm[:, 1:2], in0=sm[:, 0:1], scalar1=-SCALE, scalar2=None, op0=ALU.mult), deps=[tm])
                tb = K.op('dve', lambda e, sm=sm, qt=qt, h=h: e.tensor_scalar(out=sm[:, 8:16], in0=biasb[:, qt, h, :], scalar1=sm[:, 1:2], scalar2=None, op0=ALU.add), deps=[tm, t_bias])
                tb = K.op('dve', lambda e, sm=sm: e.memset(sm[:, 16:24], 0.0), deps=[tb])
                te = None
                for n in range(8):
                    te = K.op('act', lambda e, n=n, s=s, sm=sm: e.activation(out=Pb[s][:, n * 256:(n + 1) * 256], in_=sc[s][:, n * 256:(n + 1) * 256], func=AF.Exp,
                                                                          bias=sm[:, 8 + n:9 + n], scale=SCALE, accum_out=sm[:, 16 + n:17 + n]), deps=[tb] + Pfree[s])
                scfree[s] = [te]
                tr = K.op('dve', lambda e, sm=sm: e.tensor_reduce(out=sm[:, 24:25], in_=sm[:, 16:24], axis=AX.X, op=ALU.add), deps=[te])
                tr = K.op('dve', lambda e, sm=sm: e.reciprocal(out=sm[:, 25:26], in_=sm[:, 24:25]), deps=[tr])
                tcs = []
                tp = None
                for half in range(2):
                    b = 4 + half
                    psb = BK.bf16(b).rearrange("p (a c) -> p a c", a=8)
                    for j in range(8):
                        kt = half * 8 + j
                        tp = K.op('pe', lambda e, kt=kt, j=j, s=s, psb=psb: e.transpose(out=psb[:, j, :], in_=Pb[s][:, kt * 128:(kt + 1) * 128], identity=C['identb'][:]),
                                  deps=[te, C['t_identb'], BK.free[b]], mark=(j == 7))
                    if half == 0:
                        tcp = K.op('act', lambda e, s=s, psb=psb: e.activation(out=PT[s][:, 0:8, :], in_=psb, func=AF.Copy), deps=[tp] + PTfree[s])
                    else:
                        tcp = K.op('dve', lambda e, s=s, psb=psb: e.tensor_copy(out=PT[s][:, 8:16, :], in_=psb), deps=[tp] + PTfree[s])
                    BK.free[b] = tcp
                    tcs.append(tcp)
                Pfree[s] = [tp]
                bo = 6 + s
                tpv = None
                for kt in range(16):
                    tpv = K.op('pe', lambda e, kt=kt, s=s, bo=bo: e.matmul(BK.f32(bo)[:, 0:128], lhsT=PT[s][:, kt, :], rhs=Vh[:, kt, :], start=(kt == 0), stop=(kt == 15)),
                               deps=tcs + [t_v, BK.free[bo]], mark=(kt == 15))
                PTfree[s] = [tpv]
                vfree.append(tpv)
                t_o = K.op('act', lambda e, qt=qt, h=h, bo=bo, sm=sm: e.activation(out=otok[:, qt, h * 128:(h + 1) * 128], in_=BK.f32(bo)[:, 0:128], func=AF.Copy, scale=sm[:, 25:26]),
                           deps=[tpv, tr])
                BK.free[bo] = t_o
                smfree[s] = [t_o]
        t_oT = []
        for tt in range(8):
            for half in range(2):
                b = half
                psb = BK.bf16(b).rearrange("p (a c) -> p a c", a=8)
                tp = None
                for j in range(8):
                    kc = half * 8 + j
                    tp = K.op('pe', lambda e, kc=kc, j=j, tt=tt, psb=psb: e.transpose(out=psb[:, j, :], in_=otok[:, tt, kc * 128:(kc + 1) * 128], identity=C['identb'][:]),
                              deps=[t_o, BK.free[b]], mark=(j == 7))
                if half == 0:
                    tcp = K.op('act', lambda e, tt=tt, psb=psb: e.activation(out=hT[:, 0:8, tt * 128:(tt + 1) * 128], in_=psb, func=AF.Copy), deps=[tp, lastpe])
                else:
                    tcp = K.op('dve', lambda e, tt=tt, psb=psb: e.tensor_copy(out=hT[:, 8:16, tt * 128:(tt + 1) * 128], in_=psb), deps=[tp, lastpe])
                BK.free[b] = tcp
                t_oT.append(tcp)
        t_x = K.dma('sp', K.chan("xr2"), xT[:], T['xT_in'].rearrange("p (k t) -> p k t", k=KC), deps=[tp])
        wo = T['attn_w_o']
        t_xn = None
        for i in range(8):
            so_, Wo, tWo = ring.load(wpiece(wo, 256 * i, 256))
            lastpe2 = None
            for j in range(2):
                dc = 2 * i + j
                for half in range(2):
                    bk = 2 + (dc * 2 + half) % 4
                    for kc in range(KC):
                        lastpe2 = K.op('pe', lambda e, kc=kc, j=j, Wo=Wo, bk=bk, half=half: e.matmul(
                            BK.f32(bk), lhsT=Wo[:, kc, j * 128:(j + 1) * 128], rhs=hT[:, kc, half * 512:(half + 1) * 512], start=(kc == 0), stop=(kc == KC - 1)),
                            deps=[tWo, BK.free[bk]] + t_oT, mark=(kc == KC - 1))
                    t_xn = K.op('dve', lambda e, dc=dc, half=half, bk=bk: e.scalar_tensor_tensor(
                        out=xT[:, dc, half * 512:(half + 1) * 512], in0=BK.f32(bk), scalar=adaT[:, 1, 32 + dc:33 + dc],
                        in1=xT[:, dc, half * 512:(half + 1) * 512], op0=ALU.mult, op1=ALU.add), deps=[lastpe2, t_x])
                    BK.free[bk] = t_xn
            ring.release(so_, lastpe2)
        scratch = {'rstd': scr[:, 4096:5120], 'sq': scr[:, 0:2048].rearrange("p (a b) -> p a b", a=2),
                   'h32': scr[:, 2048:4096].rearrange("p (a b) -> p a b", a=2),
                   'hrow': scr[:, 5120:7168].bitcast(BF16).rearrange("p (s d) -> p s d", s=2)}
        K.wait('pe', [lastpe2])
        toks = moe_home_pre(K, C, BK, T, xT, adaT, featT, 1, hT, scratch, [t_xn, lastpe2])
        K.flush(final_deps=toks)


ROPE_THETA = 500000.0
DEBUG_STOP = None
DEBUG_OUT = {}
BF = ml_dtypes.bfloat16

PH_IO = {
    'p0': (dict(ident=([128, 128], F32), c_all=([4, D], F32), ada_w_sh=([2, D, 1536], F32), ada_b_sh=([2, 1536], F32)),
           dict(ada_part=([8, 1536], F32))),
    'p1': (dict(ident=([128, 128], F32), smallpack=([15, D], F32), G=([64, 1536], F32), bselc=([4, 1], F32), x_loc=([NT, D], F32),
                haloflag=([128, 1], F32), conv_w_in=([D, 3 * D], F32), conv_w_out=([D, D], F32), router_w=([2, D, E], F32), router_b=([2, E], F32)),
           dict(comb_loc=([NT, E], F32), h2_loc=([NT, D], BF16), xT_spill=([128, KC * NT], F32))),
    'pe': (dict(ident=([128, 128], F32), elcap4=([128, EL], F32), comb_all=([8192, E], F32), esel=([128, EL, E], F32), tokid16=([128, 64, 16], I32), lstrict=([128, 128], F32),
                h_all=([8192, D], BF16), w_gu_loc=([1, EL, D, 2 * D], F32), w_down_loc=([1, EL, D, D], F32), b_gu_loc=([1, EL, 2 * D], F32),
                b_down_loc=([1, EL, D], F32)),
           dict(ybuf=([EL * CAP, D], BF16), idxbuf=([EL * CAP, 16], I32))),
    'p3': (dict(ident=([128, 128], F32), smallpack=([15, D], F32), G=([64, 1536], F32), bselc=([4, 1], F32), xT_in=([128, KC * NT], F32),
                comb_all=([8192, E], F32), comb_loc=([NT, E], F32), psel=([128, 64], F32), lstrict=([128, 128], F32), ecap=([128, E], F32),
                yall=([E * CAP, D], BF16), pos_loc=([1, NT], I32), invf=([128, 1], F32), protT=([128, 128], F32), w_kv=([D, 2 * D], F32)),
           dict(xT_spill=([128, KC * NT], F32), kT_loc=([NH, 128, NT], BF16), v_loc=([NT, D], BF16), kmean_loc=([128, NH, 4], F32))),
    'p4': (dict(ident=([128, 128], F32), smallpack=([15, D], F32), G=([64, 1536], F32), bselc=([4, 1], F32), xT_in=([128, KC * NT], F32),
                pos_loc=([1, NT], I32), invf=([128, 1], F32), protT=([128, 128], F32), kmean_all=([NH, 128, 8], F32), kT_all=([NH, 128, 2 * NT], BF16),
                v_all=([2 * NT, D], BF16), cmask=([NT, 2 * NT], BF16), elig=([NT, 8], F32), own=([NT, 8], F32), attn_w_q=([D, D], F32),
                attn_w_o=([D, D], F32), router_w=([2, D, E], F32), router_b=([2, E], F32)),
           dict(comb_loc=([NT, E], F32), h2_loc=([NT, D], BF16), xT_spill=([128, KC * NT], F32))),
    'p6': (dict(ident=([128, 128], F32), smallpack=([15, D], F32), G=([64, 1536], F32), bselc=([4, 1], F32), xT_in=([128, KC * NT], F32),
                comb_all=([8192, E], F32), comb_loc=([NT, E], F32), psel=([128, 64], F32), lstrict=([128, 128], F32), ecap=([128, E], F32),
                yall=([E * CAP, D], BF16)),
           dict(out_loc=([NT, D], F32))),
}
PH_FN = {'p0': lambda nc, T: phase0(nc, T), 'p1': lambda nc, T: phase1(nc, T), 'pe': lambda nc, T: phase_expert(nc, T, 0),
         'p3': lambda nc, T: phase3(nc, T), 'p4': lambda nc, T: phase4(nc, T), 'p6': lambda nc, T: phase6(nc, T)}


def build_phase(name):
    nc = bass.Bass("TRN2", target_bir_lowering=False)
    ins, outs = PH_IO[name]
    T = {}
    for k, (shp, dt) in ins.items():
        T[k] = nc.dram_tensor(k, list(shp), dt, kind="ExternalInput").ap()
    for k, (shp, dt) in outs.items():
        T[k] = nc.dram_tensor(k, list(shp), dt, kind="ExternalOutput").ap()
    PH_FN[name](nc, T)
    return nc


def run_phase(name, in_maps):
    nc = build_phase(name)
    ins, _ = PH_IO[name]
    maps = [{k: np.ascontiguousarray(m[k]) for k in ins} for m in in_maps]
    res = run_bass_kernel_spmd(nc, maps, core_ids=list(range(NCORES)))
    return res.results


def host_consts(r):
    b, hf = r // 2, r % 2
    t0 = hf * NT
    c = {}
    c['ident'] = np.eye(128, dtype=np.float32)
    c['lstrict'] = np.triu(np.ones((128, 128), np.float32), 1)
    c['ecap'] = np.broadcast_to((np.arange(E, dtype=np.float32) * CAP)[None, :], (128, E)).copy()
    tok = (np.arange(64)[None, :] * 128 + np.arange(128)[:, None]).astype(np.int32)
    c['tokid16'] = np.broadcast_to(tok[:, :, None], (128, 64, 16)).copy()
    es = np.zeros((128, EL, E), np.float32)
    for el in range(EL):
        es[:, el, EL * r + el] = 1.0
    c['esel'] = es
    c['elcap4'] = np.broadcast_to((np.arange(EL, dtype=np.float32) * CAP)[None, :], (128, EL)).copy()
    c['psel'] = np.broadcast_to((np.arange(64) < 8 * r).astype(np.float32)[None, :], (128, 64)).copy()
    bs = np.zeros((4, 1), np.float32)
    bs[b, 0] = 1.0
    c['bselc'] = bs
    c['haloflag'] = np.full((128, 1), float(hf), np.float32)
    inv = (np.float32(ROPE_THETA) ** (-np.arange(0, 32, 2, dtype=np.float32) / np.float32(32))).astype(np.float32)
    iv = np.zeros((128, 1), np.float32)
    iv[0:32, 0] = np.concatenate([inv, inv])
    c['invf'] = iv
    pt = np.zeros((128, 128), np.float32)
    for m in range(16):
        pt[m + 16, m] = -1.0
        pt[m, m + 16] = 1.0
    c['protT'] = pt
    q = t0 + np.arange(NT)
    k = np.arange(2 * NT)
    c['cmask'] = np.where(k[None, :] > q[:, None], np.float32(-1e30), np.float32(0)).astype(BF)
    blk = q // 256
    c['elig'] = (np.arange(8)[None, :] < blk[:, None]).astype(np.float32)
    c['own'] = (np.arange(8)[None, :] == blk[:, None]).astype(np.float32)
    return c


def kernel(x, c, positions, norm_g, ada_w, ada_b, conv_w_in, conv_w, conv_w_out, kv_norm_g, w_kv, attn_w_q, attn_w_o,
           router_w, router_b, moe_w_gu, moe_b_gu, moe_w_down, moe_b_down, final_g):
    A = lambda a: np.asarray(a)
    x, c, positions = A(x), A(c), A(positions)
    HC = [host_consts(r) for r in range(NCORES)]
    base = []
    for r in range(NCORES):
        b, hf = r // 2, r % 2
        t0 = hf * NT
        m = dict(HC[r])
        halo = x[b, t0 - 2:t0, :] if hf == 1 else np.zeros((2, D), np.float32)
        m['smallpack'] = np.concatenate([A(norm_g).reshape(4, D), A(conv_w).reshape(3, D), A(kv_norm_g).reshape(1, D), A(final_g).reshape(1, D),
                                         halo, c], axis=0).astype(np.float32)
        m['c_all'] = c
        m['x_loc'] = x[b, t0:t0 + NT, :]
        m['pos_loc'] = positions[b, t0:t0 + NT].reshape(1, NT).astype(np.int32)
        m['ada_w_sh'] = A(ada_w)[:, :, r * 1536:(r + 1) * 1536]
        m['ada_b_sh'] = A(ada_b)[:, r * 1536:(r + 1) * 1536]
        m['conv_w_in'] = A(conv_w_in)[0]
        m['conv_w_out'] = A(conv_w_out)[0]
        m['w_kv'] = A(w_kv)
        m['attn_w_q'] = A(attn_w_q)[0]
        m['attn_w_o'] = A(attn_w_o)[0]
        m['router_w'] = A(router_w)
        m['router_b'] = A(router_b)
        base.append(m)
    r0 = run_phase('p0', base)
    G = np.concatenate([r0[r]['ada_part'] for r in range(NCORES)], axis=0)
    for m in base:
        m['G'] = G
    if DEBUG_STOP == 'p0':
        return r0
    r1 = run_phase('p1', base)
    if DEBUG_STOP == 'p1':
        return r1

    def moe_layer(l, rprev):
        h_all = np.concatenate([rprev[r]['h2_loc'] for r in range(NCORES)], axis=0)
        comb_all = np.concatenate([rprev[r]['comb_loc'] for r in range(NCORES)], axis=0)
        for r, m in enumerate(base):
            m['h_all'] = h_all
            m['comb_all'] = comb_all
            m['comb_loc'] = rprev[r]['comb_loc']
            m['xT_in'] = rprev[r]['xT_spill']
            m['w_gu_loc'] = A(moe_w_gu)[l:l + 1, EL * r:EL * r + EL]
            m['w_down_loc'] = A(moe_w_down)[l:l + 1, EL * r:EL * r + EL]
            m['b_gu_loc'] = A(moe_b_gu)[l:l + 1, EL * r:EL * r + EL]
            m['b_down_loc'] = A(moe_b_down)[l:l + 1, EL * r:EL * r + EL]
        re = run_phase('pe', base)
        DEBUG_OUT['pe%d' % l] = re
        yall = np.concatenate([re[r]['ybuf'] for r in range(NCORES)], axis=0)
        for m in base:
            m['yall'] = yall
            for k in ('w_gu_loc', 'w_down_loc', 'h_all'):
                m.pop(k, None)
    moe_layer(0, r1)
    if DEBUG_STOP == 'pe0':
        return DEBUG_OUT['pe0']
    r3 = run_phase('p3', base)
    if DEBUG_STOP == 'p3':
        return r3
    for p in range(4):
        a, bq = r3[2 * p], r3[2 * p + 1]
        kT_all = np.concatenate([a['kT_loc'], bq['kT_loc']], axis=2)
        v_all = np.concatenate([a['v_loc'], bq['v_loc']], axis=0)
        km = np.concatenate([a['kmean_loc'], bq['kmean_loc']], axis=2).transpose(1, 0, 2)
        for r in (2 * p, 2 * p + 1):
            base[r]['kT_all'] = kT_all
            base[r]['v_all'] = v_all
            base[r]['kmean_all'] = np.ascontiguousarray(km)
            base[r]['xT_in'] = r3[r]['xT_spill']
    r4 = run_phase('p4', base)
    if DEBUG_STOP == 'p4':
        return r4
    moe_layer(1, r4)
    r6 = run_phase('p6', base)
    out = np.zeros((4, 2 * NT, D), np.float32)
    for r in range(NCORES):
        b, hf = r // 2, r % 2
        out[b, hf * NT:(hf + 1) * NT, :] = r6[r]['out_loc']
    return out
```
